# Optimizing a Trainium2 kernel written in Bass

```python
import math
import jax
import jax.numpy as jnp
from jax import lax
import numpy as np

D_MODEL = 1024
BATCH = 8
SEQ = 4096
DEPTH = 1

GRID_W = 64
CTX_LEN = 256
EPS = 1e-6

RET_HEADS = 4
RET_QK_DIM = 128
RET_V_DIM = 256
RET_QK_WIDTH = RET_HEADS * RET_QK_DIM
RET_V_WIDTH = RET_HEADS * RET_V_DIM
RET_CHUNK = 128
ROPE_BASE = 10000.0

HY_WIDTH = 512
HY_SHORT = 3
HY_POS_DIM = 33
HY_BANDS = (HY_POS_DIM - 1) // 2
HY_FFN = 64
HY_DECAY_TARGET = 1e-2
HY_FAST_PCT = 0.3
HY_SLOW_PCT = 1.5

N_GROUPS = 4
EXPERTS_PER_GROUP = 8
N_EXPERTS = N_GROUPS * EXPERTS_PER_GROUP
TOP_K = 2
EXPERT_HIDDEN = 512
MOE_BLOCK = 128

IN_WIDTH = 2 * RET_QK_WIDTH + 2 * RET_V_WIDTH + 3 * HY_WIDTH + 2 * D_MODEL
IN_SPLITS = (RET_QK_WIDTH, 2 * RET_QK_WIDTH, 2 * RET_QK_WIDTH + RET_V_WIDTH, 2 * RET_QK_WIDTH + 2 * RET_V_WIDTH, 2 * RET_QK_WIDTH + 2 * RET_V_WIDTH + 3 * HY_WIDTH, 2 * RET_QK_WIDTH + 2 * RET_V_WIDTH + 3 * HY_WIDTH + D_MODEL)

kernel_name = 'hybrid_retention_hyena_hmoe_dit'


def rms_norm(x, gain=None):
    x32 = x.astype(jnp.float32)
    y = x32 * lax.rsqrt(jnp.mean(jnp.square(x32), axis=-1, keepdims=True) + EPS)
    if gain is not None:
        y = y * gain.astype(jnp.float32)
    return y.astype(x.dtype)


def modulate(h, shift, scale):
    return h * (1.0 + scale) + shift


def grid_rope_angles(n_tokens):
    rows = n_tokens // GRID_W
    r, col = jnp.meshgrid(jnp.arange(rows, dtype=jnp.float32), jnp.arange(GRID_W, dtype=jnp.float32), indexing='ij')
    n_freq = RET_QK_DIM // 4
    inv_freq = ROPE_BASE ** (-jnp.arange(n_freq, dtype=jnp.float32) / n_freq)
    return r.reshape(-1)[:, None] * inv_freq, col.reshape(-1)[:, None] * inv_freq


def rope_1d(x, ang):
    n_freq = ang.shape[-1]
    x1, x2 = x[..., :n_freq], x[..., n_freq:]
    cos, sin = jnp.cos(ang), jnp.sin(ang)
    return jnp.concatenate([x1 * cos - x2 * sin, x1 * sin + x2 * cos], axis=-1)


def rope_grid(x, ang_row, ang_col):
    half = RET_QK_DIM // 2
    return jnp.concatenate([rope_1d(x[..., :half], ang_row), rope_1d(x[..., half:], ang_col)], axis=-1)


def split_heads(t, n_heads):
    b, n, _ = t.shape
    return t.reshape(b, n, n_heads, -1).transpose(0, 2, 1, 3)


def retention_scan(q, k, v, log_g, s0, strict):
    b, h, n, _ = q.shape
    dv = v.shape[-1]
    n_chunks = n // RET_CHUNK
    pos = jnp.arange(RET_CHUNK, dtype=jnp.float32)
    diff = pos[:, None] - pos[None, :]
    mask = (diff > 0) if strict else (diff >= 0)
    lg = log_g[:, None, None]
    decay_in = jnp.where(mask[None], jnp.exp(lg * jnp.maximum(diff, 0.0)[None]), 0.0)
    decay_q = jnp.exp(lg * (pos + 1.0)[None, :, None])
    decay_k = jnp.exp(lg * (RET_CHUNK - 1.0 - pos)[None, :, None])
    decay_chunk = jnp.exp(lg * RET_CHUNK)

    def chunks(t):
        return t.reshape(b, h, n_chunks, RET_CHUNK, t.shape[-1]).transpose(2, 0, 1, 3, 4)

    def step(state, qkv):
        qc, kc, vc = qkv
        scores = jnp.einsum('bhid,bhjd->bhij', qc, kc) * decay_in
        out = jnp.einsum('bhij,bhjv->bhiv', scores, vc) + jnp.einsum('bhid,bhdv->bhiv', qc, state) * decay_q
        state = state * decay_chunk + jnp.einsum('bhjd,bhjv->bhdv', kc * decay_k, vc)
        return state, out

    s_final, o = lax.scan(step, s0, (chunks(q), chunks(k), chunks(v)))
    return o.transpose(1, 2, 0, 3, 4).reshape(b, h, n, dv), s_final


def retention_bidir(q, k, v, log_g, s0_fwd, s0_bwd):
    o_f, s_f = retention_scan(q, k, v, log_g[0], s0_fwd, False)
    o_b, s_b = retention_scan(jnp.flip(q, 2), jnp.flip(k, 2), jnp.flip(v, 2), log_g[1], s0_bwd, True)
    return o_f + jnp.flip(o_b, 2), s_f, s_b


def retention_readout(o, g):
    b, h, n, dv = o.shape
    o = rms_norm(o).transpose(0, 2, 1, 3).reshape(b, n, h * dv)
    return jax.nn.silu(g) * o.astype(g.dtype)


def short_conv(u, w, bias):
    n = u.shape[1]
    pad = HY_SHORT // 2
    up = jnp.pad(u, ((0, 0), (pad, pad), (0, 0)))
    return sum(up[:, j:j + n] * w[j] for j in range(HY_SHORT)) + bias


def hyena_filters(n, w1, b1, freq, w2, b2, w3):
    f32 = jnp.float32
    t = jnp.arange(n, dtype=f32) / n
    bands = jnp.linspace(1e-4, HY_BANDS - 1, HY_BANDS, dtype=f32)
    phase = 2.0 * math.pi * t[:, None] * bands[None, :]
    feats = jnp.concatenate([t[:, None], jnp.cos(phase), -jnp.sin(phase)], axis=-1)
    freq = freq.astype(f32)
    hid = jnp.sin(freq * (feats @ w1.astype(f32) + b1.astype(f32)))
    hid = jnp.sin(freq * (hid @ w2.astype(f32) + b2.astype(f32)))
    filt = hid @ w3.astype(f32)
    slow = abs(math.log(HY_DECAY_TARGET)) / HY_SLOW_PCT
    fast = abs(math.log(HY_DECAY_TARGET)) / HY_FAST_PCT
    deltas = jnp.tile(jnp.linspace(slow, fast, HY_WIDTH, dtype=f32), 2)
    filt = filt * jnp.exp(-t[:, None] * deltas[None, :])
    filt = filt / jnp.sum(jnp.abs(filt), axis=0, keepdims=True)
    return filt[:, :HY_WIDTH], filt[:, HY_WIDTH:]


def long_conv_bidir(z, h_fwd, h_bwd):
    n = z.shape[1]
    nfft = 2 * n
    zf = jnp.fft.rfft(z.astype(jnp.float32), n=nfft, axis=1)
    hf = jnp.fft.rfft(h_fwd, n=nfft, axis=0) + jnp.conj(jnp.fft.rfft(h_bwd, n=nfft, axis=0))
    return jnp.fft.irfft(zf * hf[None], n=nfft, axis=1)[:, :n]


def hyena_branch(u, conv_w, conv_b, ffn_w1, ffn_b1, ffn_freq, ffn_w2, ffn_b2, ffn_w3, skip):
    x0, x1, v = jnp.split(short_conv(u, conv_w, conv_b), 3, axis=-1)
    z = x1 * v
    h_fwd, h_bwd = hyena_filters(u.shape[1], ffn_w1, ffn_b1, ffn_freq, ffn_w2, ffn_b2, ffn_w3)
    y = long_conv_bidir(z, h_fwd, h_bwd).astype(u.dtype) + z * skip
    return x0 * y


def merge_branches(ret_gated, hy_out, gate_ret, gate_hy, ret_w_o, hy_w_o, w_out):
    mixed = jax.nn.sigmoid(gate_ret) * (ret_gated @ ret_w_o) + jax.nn.sigmoid(gate_hy) * (hy_out @ hy_w_o)
    return mixed @ w_out


def moe_ffn(u, rg_w, rg_b, re_w, re_b, w1, w3, w2):
    n_tok = u.shape[0]
    f32 = jnp.float32
    group_p = jax.nn.softmax((u @ rg_w).astype(f32) + rg_b.astype(f32), axis=-1)
    p_star, g_star = lax.top_k(group_p, 1)
    expert_logits = ((u @ re_w).astype(f32) + re_b.astype(f32)).reshape(n_tok, N_GROUPS, EXPERTS_PER_GROUP)
    in_group = jnp.take_along_axis(expert_logits, g_star[:, :, None], axis=1)[:, 0]
    w_top, e_top = lax.top_k(jax.nn.softmax(in_group, axis=-1), TOP_K)
    weights = p_star * w_top / jnp.sum(w_top, axis=-1, keepdims=True)
    expert_id = g_star * EXPERTS_PER_GROUP + e_top
    n_assign = n_tok * TOP_K
    flat_e = expert_id.reshape(n_assign)
    flat_tok = jnp.arange(n_assign, dtype=jnp.int32) // TOP_K
    flat_w = weights.reshape(n_assign)
    order = jnp.argsort(flat_e)
    se, stok, sw = flat_e[order], flat_tok[order], flat_w[order]
    counts = jnp.bincount(flat_e, length=N_EXPERTS)
    starts = jnp.cumsum(counts) - counts
    padded = (counts + MOE_BLOCK - 1) // MOE_BLOCK * MOE_BLOCK
    padded_end = jnp.cumsum(padded)
    padded_start = padded_end - padded
    dest = padded_start[se] + jnp.arange(n_assign, dtype=jnp.int32) - starts[se]
    n_blocks = -(-(n_assign + N_EXPERTS * (MOE_BLOCK - 1)) // MOE_BLOCK)
    n_rows = n_blocks * MOE_BLOCK
    row_tok = jnp.zeros((n_rows,), jnp.int32).at[dest].set(stok)
    block_expert = jnp.minimum(jnp.searchsorted(padded_end, jnp.arange(n_blocks, dtype=jnp.int32) * MOE_BLOCK, side='right'), N_EXPERTS - 1)
    xb = u[row_tok].reshape(n_blocks, MOE_BLOCK, u.shape[1])

    def expert_block(args):
        xblk, e = args
        return (jax.nn.silu(xblk @ w1[e]) * (xblk @ w3[e])) @ w2[e]

    yb = lax.map(expert_block, (xb, block_expert)).reshape(n_rows, u.shape[1])
    y = yb[dest] * sw[:, None].astype(u.dtype)
    return jax.ops.segment_sum(y, stok, num_segments=n_tok)


def setup_inputs(seed: int = 0) -> dict:
    key = jax.random.key(seed)
    ks = jax.random.split(key, 32)
    f32 = jnp.float32

    def nrm(k, shape, scale):
        return scale * jax.random.normal(k, shape, f32)

    base_logit = np.log(2.0 ** (5 + np.arange(RET_HEADS)) - 1.0).astype(np.float32)
    return {
        'x': nrm(ks[0], (BATCH, SEQ, D_MODEL), 1.0),
        'c': nrm(ks[1], (BATCH, D_MODEL), 1.0),
        'ctx': nrm(ks[2], (BATCH, CTX_LEN, D_MODEL), 1.0),
        'c_ctx': nrm(ks[3], (D_MODEL,), 1.0),
        'ada_w': nrm(ks[4], (DEPTH, D_MODEL, 6 * D_MODEL), 0.5 * D_MODEL ** -0.5),
        'ada_b': nrm(ks[5], (DEPTH, 6 * D_MODEL), 0.02),
        'norm1_g': 1.0 + nrm(ks[6], (DEPTH, D_MODEL), 0.05),
        'norm2_g': 1.0 + nrm(ks[7], (DEPTH, D_MODEL), 0.05),
        'w_in': nrm(ks[8], (DEPTH, D_MODEL, IN_WIDTH), D_MODEL ** -0.5),
        'b_in': nrm(ks[9], (DEPTH, IN_WIDTH), 0.02),
        'ret_decay_logit': jnp.asarray(base_logit)[None, None, :] + nrm(ks[10], (DEPTH, 2, RET_HEADS), 0.1),
        'ret_w_o': nrm(ks[11], (DEPTH, RET_V_WIDTH, D_MODEL), RET_V_WIDTH ** -0.5),
        'hy_conv_w': nrm(ks[12], (DEPTH, HY_SHORT, 3 * HY_WIDTH), HY_SHORT ** -0.5),
        'hy_conv_b': nrm(ks[13], (DEPTH, 3 * HY_WIDTH), 0.02),
        'hy_ffn_w1': nrm(ks[14], (DEPTH, HY_POS_DIM, HY_FFN), HY_POS_DIM ** -0.5),
        'hy_ffn_b1': nrm(ks[15], (DEPTH, HY_FFN), 0.02),
        'hy_ffn_freq': 1.0 + nrm(ks[16], (DEPTH, HY_FFN), 0.05),
        'hy_ffn_w2': nrm(ks[17], (DEPTH, HY_FFN, HY_FFN), HY_FFN ** -0.5),
        'hy_ffn_b2': nrm(ks[18], (DEPTH, HY_FFN), 0.02),
        'hy_ffn_w3': nrm(ks[19], (DEPTH, HY_FFN, 2 * HY_WIDTH), HY_FFN ** -0.5),
        'hy_skip': nrm(ks[20], (DEPTH, HY_WIDTH), 0.5),
        'hy_w_o': nrm(ks[21], (DEPTH, HY_WIDTH, D_MODEL), HY_WIDTH ** -0.5),
        'w_out': nrm(ks[22], (DEPTH, D_MODEL, D_MODEL), D_MODEL ** -0.5),
        'router_group_w': nrm(ks[23], (DEPTH, D_MODEL, N_GROUPS), D_MODEL ** -0.5),
        'router_group_b': nrm(ks[24], (DEPTH, N_GROUPS), 0.01),
        'router_expert_w': nrm(ks[25], (DEPTH, D_MODEL, N_EXPERTS), D_MODEL ** -0.5),
        'router_expert_b': nrm(ks[26], (DEPTH, N_EXPERTS), 0.01),
        'expert_w1': nrm(ks[27], (DEPTH, N_EXPERTS, D_MODEL, EXPERT_HIDDEN), D_MODEL ** -0.5),
        'expert_w3': nrm(ks[28], (DEPTH, N_EXPERTS, D_MODEL, EXPERT_HIDDEN), D_MODEL ** -0.5),
        'expert_w2': nrm(ks[29], (DEPTH, N_EXPERTS, EXPERT_HIDDEN, D_MODEL), EXPERT_HIDDEN ** -0.5),
        'final_norm_g': 1.0 + nrm(ks[30], (D_MODEL,), 0.05),
    }


def reference(x, c, ctx, c_ctx, ada_w, ada_b, norm1_g, norm2_g, w_in, b_in, ret_decay_logit, ret_w_o, hy_conv_w, hy_conv_b, hy_ffn_w1, hy_ffn_b1, hy_ffn_freq, hy_ffn_w2, hy_ffn_b2, hy_ffn_w3, hy_skip, hy_w_o, w_out, router_group_w, router_group_b, router_expert_w, router_expert_b, expert_w1, expert_w3, expert_w2, final_norm_g):
    f32 = jnp.float32
    batch, n_tokens = x.shape[0], x.shape[1]
    ang_row, ang_col = grid_rope_angles(n_tokens)
    silu_c = jax.nn.silu(c)
    silu_cc = jax.nn.silu(c_ctx)
    k_scale = RET_QK_DIM ** -0.5
    for layer in range(DEPTH):
        last = layer == DEPTH - 1
        sh1, sc1, gt1, sh2, sc2, gt2 = jnp.split((silu_c @ ada_w[layer] + ada_b[layer])[:, None, :], 6, axis=-1)
        csh1, csc1, cgt1, csh2, csc2, cgt2 = jnp.split(silu_cc @ ada_w[layer] + ada_b[layer], 6, axis=-1)
        h = modulate(rms_norm(x, norm1_g[layer]), sh1, sc1)
        hc = modulate(rms_norm(ctx, norm1_g[layer]), csh1, csc1)
        q, k, v, g, hy_u, gate_ret, gate_hy = jnp.split(h @ w_in[layer] + b_in[layer], IN_SPLITS, axis=-1)
        qc, kc, vc, gc, hyc_u, gate_ret_c, gate_hy_c = jnp.split(hc @ w_in[layer] + b_in[layer], IN_SPLITS, axis=-1)
        log_g = jax.nn.log_sigmoid(ret_decay_logit[layer].astype(f32))
        zero_state = jnp.zeros((batch, RET_HEADS, RET_QK_DIM, RET_V_DIM), f32)
        o_ctx, s_fwd, s_bwd = retention_bidir(split_heads(qc, RET_HEADS).astype(f32), split_heads(kc, RET_HEADS).astype(f32) * k_scale, split_heads(vc, RET_HEADS).astype(f32), log_g, zero_state, zero_state)
        q_h = rope_grid(split_heads(q, RET_HEADS).astype(f32), ang_row, ang_col)
        k_h = rope_grid(split_heads(k, RET_HEADS).astype(f32), ang_row, ang_col) * k_scale
        o_lat, _, _ = retention_bidir(q_h, k_h, split_heads(v, RET_HEADS).astype(f32), log_g, s_fwd, s_bwd)
        hy_args = (hy_conv_w[layer], hy_conv_b[layer], hy_ffn_w1[layer], hy_ffn_b1[layer], hy_ffn_freq[layer], hy_ffn_w2[layer], hy_ffn_b2[layer], hy_ffn_w3[layer], hy_skip[layer])
        out_args = (ret_w_o[layer], hy_w_o[layer], w_out[layer])
        moe_args = (router_group_w[layer], router_group_b[layer], router_expert_w[layer], router_expert_b[layer], expert_w1[layer], expert_w3[layer], expert_w2[layer])
        mix = merge_branches(retention_readout(o_lat, g), hyena_branch(hy_u, *hy_args), gate_ret, gate_hy, *out_args)
        x = x + gt1 * mix
        h2 = modulate(rms_norm(x, norm2_g[layer]), sh2, sc2)
        if last:
            x = x + gt2 * moe_ffn(h2.reshape(-1, D_MODEL), *moe_args).reshape(x.shape)
        else:
            mix_c = merge_branches(retention_readout(o_ctx, gc), hyena_branch(hyc_u, *hy_args), gate_ret_c, gate_hy_c, *out_args)
            ctx = ctx + cgt1 * mix_c
            h2c = modulate(rms_norm(ctx, norm2_g[layer]), csh2, csc2)
            y = moe_ffn(jnp.concatenate([h2.reshape(-1, D_MODEL), h2c.reshape(-1, D_MODEL)], axis=0), *moe_args)
            n_lat = batch * n_tokens
            x = x + gt2 * y[:n_lat].reshape(x.shape)
            ctx = ctx + cgt2 * y[n_lat:].reshape(ctx.shape)
    return rms_norm(x, final_norm_g)
```

```python
import contextlib
import math
import numpy as np
import ml_dtypes
import concourse.bass as bass
import concourse.mybir as mybir
from concourse.bass_utils import run_bass_kernel_spmd

F32 = mybir.dt.float32
BF16 = mybir.dt.bfloat16
I32 = mybir.dt.int32
U8 = mybir.dt.uint8
AF = mybir.ActivationFunctionType
ALU = mybir.AluOpType
AX = mybir.AxisListType
BF = ml_dtypes.bfloat16

L = 4096
D = 1024
NT = 32
EPS = 1e-6
INW = 6656
NSLOT = 64
SLOTR = 256
NROWS = NSLOT * SLOTR

ENGS = ("pe", "act", "dve", "pool", "sp")
NDMASEM = 8
SAME_ENGINE_WAR_SYNC = True
QSIZE = {"bg": 16}


class Sched:
    def __init__(self, nc, same_engine_sync=True):
        self.nc = nc
        self.ops = []
        self.lastw = {}
        self.readers = {}
        self.same_engine_sync = same_engine_sync
        self.bar_deps = set()
        self.bar_pending = {}

    def add(self, eng, fn, r=(), w=(), dma=False, q=None, bg=False):
        deps = set()
        war = set()
        for t in r:
            if t in self.lastw:
                deps.add(self.lastw[t])
        for t in w:
            if t in self.lastw:
                deps.add(self.lastw[t])
            for x in self.readers.get(t, ()):
                if x not in deps:
                    war.add(x)
        war -= deps
        deps |= war
        idx = len(self.ops)
        if self.bar_pending.get(eng, False):
            deps |= self.bar_deps
            self.bar_pending[eng] = False
        deps.discard(idx)
        self.ops.append(dict(eng=eng, fn=fn, deps=deps, war=war, dma=dma, sig=False, q=(q or eng), bg=bg))
        for t in r:
            self.readers.setdefault(t, []).append(idx)
        for t in w:
            self.lastw[t] = idx
            self.readers[t] = []
        return idx

    def barrier(self, include_bg=False):
        last = {}
        dmas = {}
        for i, o in enumerate(self.ops):
            if o["dma"]:
                if o["bg"] and not include_bg:
                    continue
                dmas.setdefault(o["q"], []).append(i)
            else:
                last[o["eng"]] = i
        bd = set(last.values())
        for e, l in dmas.items():
            bd |= set(l[-QSIZE.get(e, NDMASEM):])
        self.bar_deps = bd
        self.bar_pending = {e: True for e in ENGS}
        self.lastw = {}
        self.readers = {}

    def emit(self, final_wait_ops=()):
        nc = self.nc
        ops = self.ops
        for i, o in enumerate(ops):
            nd = set()
            for d in o["deps"]:
                od = ops[d]
                if od["eng"] == o["eng"] and not od["dma"]:
                    if o["eng"] in ("pe", "sp"):
                        continue
                    if not self.same_engine_sync:
                        continue
                    if d in o["war"] and not SAME_ENGINE_WAR_SYNC:
                        continue
                nd.add(d)
            o["deps"] = nd
            for d in nd:
                ops[d]["sig"] = True
        for d in final_wait_ops:
            ops[d]["sig"] = True
        cnt = {e: 0 for e in ENGS}
        dcnt = {}
        for o in ops:
            if o["dma"]:
                o["dn"] = dcnt.get(o["q"], 0)
                dcnt[o["q"]] = o["dn"] + 1
            elif o["sig"]:
                cnt[o["eng"]] += 1
                o["sn"] = cnt[o["eng"]]
        with contextlib.ExitStack() as es:
            csem = {e: es.enter_context(nc.semaphore("c_" + e)) for e in ENGS if e != "sp"}
            dsem = {e: [es.enter_context(nc.semaphore("d_%s%d" % (e, i))) for i in range(QSIZE.get(e, NDMASEM))]
                    for e in dcnt}
            block = es.enter_context(nc.Block())

            def target(d):
                od = ops[d]
                if od["dma"]:
                    n = od["dn"]
                    K_ = QSIZE.get(od["q"], NDMASEM)
                    return dsem[od["q"]][n % K_], 16 * (n // K_ + 1)
                return csem[od["eng"]], od["sn"]

            def run(ename, eobj):
                seen = {}
                for i, o in enumerate(ops):
                    if o["eng"] != ename:
                        continue
                    waits = {}
                    for d in o["deps"]:
                        s, v = target(d)
                        key = id(s)
                        if seen.get(key, 0) >= v:
                            continue
                        if key not in waits or waits[key][1] < v:
                            waits[key] = (s, v)
                    K_ = QSIZE.get(o["q"], NDMASEM)
                    if o["dma"] and o["dn"] >= K_:
                        s = dsem[o["q"]][o["dn"] % K_]
                        v = 16 * (o["dn"] // K_)
                        key = id(s)
                        if seen.get(key, 0) < v and (key not in waits or waits[key][1] < v):
                            waits[key] = (s, v)
                    for key, (s, v) in waits.items():
                        eobj.wait_ge(s, v)
                        seen[key] = v
                    ins = o["fn"](eobj)
                    if o["dma"]:
                        s, v = target(i)
                        ins.then_inc(s, 16)
                    elif o["sig"]:
                        ins.then_inc(csem[ename], 1)
                if ename == "sp":
                    for d in final_wait_ops:
                        s, v = target(d)
                        eobj.wait_ge(s, v)

            @block.sync
            def _(e):
                run("sp", e)

            @block.scalar
            def _(e):
                run("act", e)

            @block.vector
            def _(e):
                run("dve", e)

            @block.gpsimd
            def _(e):
                run("pool", e)

            @block.tensor
            def _(e):
                run("pe", e)


def _fft_tables():
    j = np.arange(128)[:, None]
    k1 = np.arange(128)[None, :]
    phi = 2 * np.pi * (k1 + 0.5) * j / 256.0
    E1c = np.cos(phi)
    E1s = -np.sin(phi)
    a = np.arange(32)[:, None]
    k2 = np.arange(32)[None, :]
    FT = np.zeros((3, 32, 128, 128))
    for q in range(32):
        for m in range(4):
            kk = 4 * q + m
            th = -2 * np.pi * ((kk + 0.5) * a / 8192.0 + k2 * a / 32.0)
            sl = slice(32 * m, 32 * m + 32)
            FT[0, q, sl, sl] = np.cos(th)
            FT[1, q, sl, sl] = np.sin(th)
            FT[2, q, sl, sl] = -np.sin(th)
    IT = np.transpose(FT, (0, 1, 3, 2)).copy()
    return E1c, E1s, FT, IT


CA = {}
CR = {}
CB = {}


def _pack(specs, table):
    off = 0
    cols = []
    for name, arr in specs:
        arr = np.asarray(arr, np.float64).reshape(128, -1)
        table[name] = (off, arr.shape[1])
        off += arr.shape[1]
        cols.append(arr)
    return np.concatenate(cols, axis=1)


_CONST_CACHE = {}


def host_consts():
    if _CONST_CACHE:
        return _CONST_CACHE
    p = np.arange(128)
    ident = np.eye(128)
    partner = np.where((p % 64) < 32, p + 32, p - 32)
    perm = np.zeros((128, 128))
    perm[partner, p] = 1.0
    jj = p[:, None]
    ii = p[None, :]
    diff_f = np.maximum(ii - jj, 0)
    mask_f = (ii >= jj) * 1.0
    diff_b = np.maximum(jj - ii, 0)
    mask_b = (jj > ii) * 1.0
    pcol = np.stack([127 - p, p], 1)
    prow = np.concatenate([np.tile(p + 1, (128, 1)), np.tile(128 - p, (128, 1))], 1)
    slow = abs(math.log(1e-2)) / 1.5
    fast = abs(math.log(1e-2)) / 0.3
    deltas = np.tile(np.linspace(slow, fast, 512, dtype=np.float32).astype(np.float64), 2)
    negdelta = -deltas.reshape(8, 128).T
    eidx = np.tile(np.arange(32), (128, 1))
    cA = _pack([("identf", ident), ("permf", perm), ("diff_f", diff_f), ("mask_f", mask_f), ("diff_b", diff_b),
                ("mask_b", mask_b), ("pcol", pcol), ("prow", prow), ("negdelta", negdelta), ("eidx", eidx),
                ("pidx", p[:, None]), ("onesf", np.ones((128, 128)))], CA).astype(np.float32)
    thr = np.tile((SLOTR * np.arange(16))[None, None, :], (128, 32, 1))
    le = np.tile((np.arange(32)[None, :] <= np.arange(32)[:, None])[None] * 1.0, (128, 1, 1))
    lt = np.tile((np.arange(32)[None, :] < np.arange(32)[:, None])[None] * 1.0, (128, 1, 1))
    slotv = np.tile((SLOTR * np.arange(NSLOT))[None, :, None], (128, 1, 32))
    cR = _pack([("thr", thr), ("le", le), ("lt", lt), ("slotv", slotv)], CR).astype(np.float32)
    E1c, E1s, FT, IT = _fft_tables()
    tri = (jj < ii) * 1.0
    cB = _pack([("identb", ident), ("tri", tri), ("onesb", np.ones((128, 128))), ("E1c", E1c), ("E1s", E1s),
                ("E1cT", E1c.T), ("E1sT", E1s.T)], CB).astype(BF)
    ftab = np.transpose(FT, (2, 0, 1, 3)).reshape(128, 3 * 32 * 128).astype(BF)
    itab = np.transpose(IT, (2, 0, 1, 3)).reshape(128, 3 * 32 * 128).astype(BF)
    rows = L // 64
    r, col = np.meshgrid(np.arange(rows, dtype=np.float32), np.arange(64, dtype=np.float32), indexing="ij")
    inv_freq = (np.float32(10000.0) ** (-np.arange(32, dtype=np.float32) / np.float32(32))).astype(np.float32)
    ang_row = (r.reshape(-1)[:, None] * inv_freq).astype(np.float32)
    ang_col = (col.reshape(-1)[:, None] * inv_freq).astype(np.float32)
    cos_t = np.concatenate([np.cos(ang_row), np.cos(ang_row), np.cos(ang_col), np.cos(ang_col)], 1).T
    sin_t = np.concatenate([-np.sin(ang_row), np.sin(ang_row), -np.sin(ang_col), np.sin(ang_col)], 1).T
    rope = np.concatenate([cos_t, sin_t], 1).astype(np.float32)
    t = (np.arange(L, dtype=np.float32) / np.float32(L)).astype(np.float32)
    bands = np.linspace(1e-4, 15, 16, dtype=np.float32)
    phase = (2.0 * math.pi * t[:, None] * bands[None, :]).astype(np.float32)
    feats = np.concatenate([t[:, None], np.cos(phase), -np.sin(phase)], -1).astype(np.float32)
    featsT = np.ascontiguousarray(feats.T)
    tvec = np.tile(t[None, :], (128, 1)).astype(np.float32)
    _CONST_CACHE.update(cA=cA, cR=cR, cB=cB, ftab=ftab, itab=itab, rope=rope, featsT=featsT, tvec=tvec)
    return _CONST_CACHE


def build_program(debug=False):
    host_consts()
    nc = bass.Bass("TRN2", target_bir_lowering=False)
    dk = "ExternalOutput" if debug else "Internal"

    def din(name, shape, dt=F32):
        return nc.dram_tensor(name, list(shape), dt, kind="ExternalInput").ap()

    def dscr(name, shape, dt=F32, dbg=False):
        return nc.dram_tensor(name, list(shape), dt, kind=(dk if dbg else "Internal")).ap()

    x_d = din("x", [L, D])
    ctx_d = din("ctx", [256, D])
    cvec_d = din("cvec", [128, 16])
    ada_w_d = din("ada_w", [D, 6144])
    ada_b_d = din("ada_b_b", [128, 6144])
    gains_d = din("gains_b", [128, 3 * D])
    w_in_d = din("w_in", [D, INW])
    b_in_fm_d = din("b_in_fm", [128, 52])
    b_in_b_d = din("b_in_b", [128, 2560])
    dlog_d = din("dlog_b", [128, 8])
    ret_w_o_d = din("ret_w_o", [D, D])
    hy_w_o_d = din("hy_w_o", [512, D])
    w_out_d = din("w_out", [D, D])
    hyc_d = din("hyc", [128, 12 * 5])
    hyskip_d = din("hyskip", [128, 4])
    hw1_d = din("hw1", [33, 64])
    hvec_d = din("hvec", [64, 4])
    hw2_d = din("hw2", [64, 64])
    hw3_d = din("hw3", [64, 1024])
    wr_d = din("wr", [D, 36])
    br_d = din("br_b", [128, 36])
    ew1_d = din("ew1", [32 * 128, 8 * 512])
    ew3_d = din("ew3", [32 * 128, 8 * 512])
    ew2_d = din("ew2", [32 * 512, D])
    cA_d = din("cA", [128, host_consts()["cA"].shape[1]])
    cR_d = din("cR", [128, host_consts()["cR"].shape[1]])
    cB_d = din("cB", [128, host_consts()["cB"].shape[1]], BF16)
    ftab_d = din("ftab", [128, 3 * 32 * 128], BF16)
    itab_d = din("itab", [128, 3 * 32 * 128], BF16)
    rope_d = din("rope", [128, 2 * L])
    featsT_d = din("featsT", [33, L])
    tvec_d = din("tvec", [128, L])
    out_d = nc.dram_tensor("out", [L, D], F32, kind="ExternalOutput").ap()

    qT_d = dscr("qT_s", [4, 128, L], BF16, True)
    kT_d = dscr("kT_s", [4, 128, L], BF16, True)
    kt_d = dscr("kt_s", [L, 512], BF16)
    v_d = dscr("v_s", [L, D], BF16, True)
    gs_d = dscr("gs_s", [L, D], BF16, True)
    u_d = dscr("u_s", [1536, L], BF16, True)
    gates_d = dscr("gates_s", [2048, L], BF16, True)
    rg_d = dscr("rg_s", [L, D], BF16, True)
    filt_d = dscr("filt_s", [1024, L], F32, True)
    cd_d = dscr("cd_s", [2, 128 * 32 * 512], BF16)
    hd_d = dscr("hd_s", [2, 128, 32 * 512], F32)
    hy_d = dscr("hy_s", [512, L], BF16, True)
    x2_d = dscr("x2_s", [L, D], F32, True)
    xb_d = dscr("xb_s", [NROWS, D], BF16)
    yb_d = dscr("yb_s", [NROWS, D], F32)
    dbg_d = dscr("dbg_s", [128, 4096], F32, True)
    mod_d = dscr("mod_s", [128, 8192], F32)
    z_d = dscr("z_s", [512, L], BF16)
    hs_d = dscr("hs_s", [2, 512, L], BF16)
    ew1b_d = dscr("ew1b_s", [32 * 128, 8 * 512], BF16)
    ew3b_d = dscr("ew3b_s", [32 * 128, 8 * 512], BF16)
    ew2b_d = dscr("ew2b_s", [32 * 512, D], BF16)

    ARENA = 204 * 1024
    with contextlib.ExitStack() as es:
        arena = es.enter_context(nc.sbuf_tensor("arena", [128, ARENA], U8))
        ps = [es.enter_context(nc.psum_tensor("ps%d" % i, [128, 512], F32)) for i in range(8)]
        psb = [p_[:, :].bitcast(BF16) for p_ in ps]
        S = Sched(nc)
        st = dict(off=0, pers=0, bank=0)

        def alloc(n, dt, pers=False):
            sz = 2 if dt == BF16 else 4
            nb_ = (n * sz + 63) // 64 * 64
            a = arena[:, st["off"]:st["off"] + n * sz].bitcast(dt)
            st["off"] += nb_
            assert st["off"] <= ARENA, ("SBUF arena overflow", st["off"])
            if pers:
                st["pers"] = st["off"]
            return a

        def phase_reset():
            S.barrier()
            st["off"] = st["pers"]

        def nb():
            st["bank"] = (st["bank"] + 1) % 8
            return st["bank"]

        def dma(eng, out, in_, r=(), w=()):
            return S.add(eng, lambda e: e.dma_start(out=out, in_=in_), r=r, w=w, dma=True)

        def op(eng, f, r=(), w=()):
            return S.add(eng, f, r=r, w=w)

        def mm(out, lhsT, rhs, start, stop):
            return lambda e: e.matmul(out, lhsT=lhsT, rhs=rhs, start=start, stop=stop)

        def mmgroup(lst):
            def f(e):
                ins = None
                for (o, l, r_, s0, s1) in lst:
                    ins = e.matmul(o, lhsT=l, rhs=r_, start=s0, stop=s1)
                return ins
            return f

        def trgroup(lst, ident):
            def f(e):
                ins = None
                for (o, i_) in lst:
                    ins = e.transpose(out=o, in_=i_, identity=ident)
                return ins
            return f

        def act(out, in_, func, r, w, bias=None, scale=None, accum=None, eng="act"):
            kw = {}
            if bias is not None:
                kw["bias"] = bias
            if scale is not None:
                kw["scale"] = scale
            if accum is not None:
                kw["accum_out"] = accum
            return S.add(eng, lambda e: e.activation(out=out, in_=in_, func=func, **kw), r=r, w=w)

        def tt(eng, out, in0, in1, o, r, w):
            return S.add(eng, lambda e: e.tensor_tensor(out=out, in0=in0, in1=in1, op=o), r=r, w=w)

        def ts(eng, out, in0, s1, s2, o0, o1, r, w):
            if s2 is None:
                return S.add(eng, lambda e: e.tensor_scalar(out=out, in0=in0, scalar1=s1, scalar2=None, op0=o0), r=r, w=w)
            return S.add(eng, lambda e: e.tensor_scalar(out=out, in0=in0, scalar1=s1, scalar2=s2, op0=o0, op1=o1), r=r, w=w)

        def stt(eng, out, in0, sc, in1, o0, o1, r, w):
            return S.add(eng, lambda e: e.scalar_tensor_tensor(out=out, in0=in0, scalar=sc, in1=in1, op0=o0, op1=o1), r=r, w=w)

        def cp(eng, out, in_, r, w):
            if eng == "act":
                return S.add(eng, lambda e: e.copy(out=out, in_=in_), r=r, w=w)
            return S.add(eng, lambda e: e.tensor_copy(out=out, in_=in_), r=r, w=w)

        def rstd_from_ss(ss, n, tag):
            act(ss, ss, AF.Sqrt, r=[tag], w=[tag], scale=1.0 / n, bias=epsc)
            op("dve", lambda e: e.reciprocal(out=ss, in_=ss), r=[tag], w=[tag])

        cA = alloc(host_consts()["cA"].shape[1], F32, pers=True)
        cBt = alloc(host_consts()["cB"].shape[1], BF16, pers=True)
        epsc = alloc(1, F32, pers=True)
        dec = alloc(8 + 8 + 8, F32, pers=True)
        s0 = alloc(8 * 256, F32, pers=True)
        dest = alloc(64, I32, pers=True)
        wts = alloc(64, F32, pers=True)
        idxw = alloc(NSLOT * 5, I32, pers=True)
        Dm = alloc(4 * 128, F32, pers=True).rearrange("p (h i) -> p h i", h=4)
        DQf = alloc(8 * 128, F32, pers=True).rearrange("p (h i) -> p h i", h=8)
        DK = alloc(8, F32, pers=True)

        def ca(name):
            o, n = CA[name]
            return cA[:, o:o + n]

        def cb(name):
            o, n = CB[name]
            return cBt[:, o:o + n]
        dma("sp", cA, cA_d, w=["cA"])
        dma("sp", cBt, cB_d, w=["cB"])
        op("dve", lambda e: e.memset(epsc, EPS), w=["epsc"])
        identb = cb("identb")
        identf = ca("identf")

        modL = alloc(6144, F32)
        gains = alloc(3 * D, F32)
        dma("sp", gains, gains_d, w=["gains"])
        cv = alloc(16, F32)
        dma("sp", cv, cvec_d, w=["cv"])
        sg = alloc(16, F32)
        act(sg, cv, AF.Sigmoid, r=["cv"], w=["sg"])
        tt("dve", cv, cv, sg, ALU.mult, r=["cv", "sg"], w=["cv"])
        rep = alloc(8 * 128 * 2, F32).rearrange("p (k j m) -> p k j m", k=8, j=2)
        for kc in range(8):
            for j_ in range(2):
                ts("dve", rep[:, kc, j_, :], ca("onesf"), cv[:, 2 * kc + j_:2 * kc + j_ + 1], None, ALU.mult, None,
                   r=["cv", "cA"], w=[("rep", kc, j_)])
        modC = alloc(2048, F32, pers=False)
        adab = alloc(6144, F32)
        dma("sp", adab, ada_b_d, w=["adab"])
        awt = [alloc(2048, F32), alloc(2048, F32)]
        ada_v = ada_w_d.rearrange("(kc p) n -> p kc n", p=128)
        for pa in range(3):
            banks = [nb() for _ in range(8 if pa == 0 else 4)]
            for kc in range(8):
                b_ = awt[kc % 2]
                dma("sp", b_, ada_v[:, kc, pa * 2048:(pa + 1) * 2048], w=[("awt", kc % 2)])
                lst = []
                for n4 in range(4):
                    lst.append((ps[banks[n4]][:, :], rep[:, kc, 0, :], b_[:, n4 * 512:(n4 + 1) * 512], kc == 0, kc == 7))
                    if pa == 0:
                        lst.append((ps[banks[4 + n4]][:, :], rep[:, kc, 1, :], b_[:, n4 * 512:(n4 + 1) * 512], kc == 0, kc == 7))
                op("pe", mmgroup(lst), r=[("awt", kc % 2)] + [("rep", kc, 0), ("rep", kc, 1)], w=[("ps", b) for b in banks])
            for n4 in range(4):
                c0 = pa * 2048 + n4 * 512
                tt("dve", modL[:, c0:c0 + 512], ps[banks[n4]][:, :], adab[:, c0:c0 + 512], ALU.add,
                   r=[("ps", banks[n4]), "adab"], w=["modL"])
                if pa == 0:
                    tt("dve", modC[:, c0:c0 + 512], ps[banks[4 + n4]][:, :], adab[:, c0:c0 + 512], ALU.add,
                       r=[("ps", banks[4 + n4]), "adab"], w=["modC"])
        A1 = modL[:, 1024:2048]
        B1 = modL[:, 0:1024]
        stt("dve", A1, A1, 1.0, gains[:, 0:1024], ALU.add, ALU.mult, r=["modL", "gains"], w=["modL"])
        A1c = modC[:, 1024:2048]
        B1c = modC[:, 0:1024]
        stt("dve", A1c, A1c, 1.0, gains[:, 0:1024], ALU.add, ALU.mult, r=["modC", "gains"], w=["modC"])
        A2 = modL[:, 4096:5120]
        B2 = modL[:, 3072:4096]
        stt("dve", A2, A2, 1.0, gains[:, 1024:2048], ALU.add, ALU.mult, r=["modL", "gains"], w=["modL"])
        dma("sp", mod_d[:, 0:6144], modL, r=["modL"], w=["mod_d"])
        dma("sp", mod_d[:, 6144:8192], modC, r=["modC"], w=["mod_d"])
        dl = alloc(8, F32)
        dma("sp", dl, dlog_d, w=["dl"])
        act(dl, dl, AF.Exp, r=["dl"], w=["dl"], scale=-1.0)
        act(dl, dl, AF.Ln, r=["dl"], w=["dl"], bias=1.0)
        LG = dec[:, 0:8]
        G128 = dec[:, 8:16]
        ts("dve", LG, dl, -1.0, None, ALU.mult, None, r=["dl"], w=["dec"])
        act(G128, LG, AF.Exp, r=["dec"], w=["dec"], scale=128.0)
        tmpd = alloc(128, F32)
        for h in range(4):
            act(tmpd, ca("diff_f"), AF.Exp, r=["cA", "dec"], w=["tmpd"], scale=LG[:, h:h + 1])
            tt("dve", Dm[:, h, :], tmpd, ca("mask_f"), ALU.mult, r=["tmpd", "cA"], w=["Dm"])
            act(tmpd, ca("diff_b"), AF.Exp, r=["cA", "dec"], w=["tmpd"], scale=LG[:, 4 + h:5 + h])
            tt("dve", tmpd, tmpd, ca("mask_b"), ALU.mult, r=["tmpd", "cA"], w=["tmpd"])
            tt("dve", Dm[:, h, :], Dm[:, h, :], tmpd, ALU.add, r=["tmpd", "Dm"], w=["Dm"])
            po, _ = CA["prow"]
            act(DQf[:, h, :], cA[:, po:po + 128], AF.Exp, r=["cA", "dec"], w=["DQ"], scale=LG[:, h:h + 1])
            act(DQf[:, 4 + h, :], cA[:, po + 128:po + 256], AF.Exp, r=["cA", "dec"], w=["DQ"], scale=LG[:, 4 + h:5 + h])
            pc, _ = CA["pcol"]
            act(DK[:, h:h + 1], cA[:, pc:pc + 1], AF.Exp, r=["cA", "dec"], w=["DK"], scale=LG[:, h:h + 1])
            act(DK[:, 4 + h:5 + h], cA[:, pc + 1:pc + 2], AF.Exp, r=["cA", "dec"], w=["DK"], scale=LG[:, 4 + h:5 + h])

        phase_reset()
        A1c = alloc(D, F32)
        B1c = alloc(D, F32)
        dma("sp", B1c, mod_d[:, 6144:7168], r=["mod_d"], w=["modC"])
        dma("sp", A1c, mod_d[:, 7168:8192], r=["mod_d"], w=["modC"])
        w_in = alloc(8 * 1536, BF16).rearrange("p (k n) -> p k n", k=8)
        w_in_v = w_in_d.rearrange("(kc p) n -> p kc n", p=128)
        for kc in range(8):
            dma("pool", w_in[:, kc, :], w_in_v[:, kc, 512:2048], w=["w_in"])
        bbb = alloc(2560, F32)
        dma("sp", bbb, b_in_b_d, w=["bbb"])
        xt = [alloc(D, F32), alloc(D, F32)]
        tmpf = alloc(D, F32)
        hb = [alloc(D, BF16), alloc(D, BF16)]
        hT = alloc(8 * 512, BF16).rearrange("p (k t) -> p k t", k=8)
        ssq = alloc(4, F32)
        junk = alloc(D, BF16)
        HT = dict(hT=hT, xt=xt, tmpf=tmpf, hb=hb, ssq=ssq, junk=junk)

        def norm_mod_T(src_ap, i, A_, B_, atag, tcol, ntile_tag):
            hT, xt, tmpf, hb, ssq, junk = HT["hT"], HT["xt"], HT["tmpf"], HT["hb"], HT["ssq"], HT["junk"]
            b = i % 2
            dma("sp", xt[b], src_ap, w=[("xt", b)])
            sscol = ssq[:, b:b + 1]
            act(junk, xt[b], AF.Square, r=[("xt", b)], w=["junk", ("ssq", b)], accum=sscol)
            rstd_from_ss(sscol, D, ("ssq", b))
            stt("dve", tmpf, xt[b], sscol, A_, ALU.mult, ALU.mult, r=[("xt", b), ("ssq", b), atag], w=["tmpf"])
            tt("dve", hb[b], tmpf, B_, ALU.add, r=["tmpf", atag], w=[("hb", b)])
            bk = nb()
            op("pe", trgroup([(psb[bk][:, kc * 128:(kc + 1) * 128], hb[b][:, kc * 128:(kc + 1) * 128]) for kc in range(8)], identb),
               r=[("hb", b), "cB"], w=[("ps", bk)])
            cp("act", hT[:, :, tcol * 128:(tcol + 1) * 128], psb[bk][:, :].rearrange("p (k t) -> p k t", k=8),
               r=[("ps", bk)], w=["hT"])

        for i in range(2):
            norm_mod_T(ctx_d[i * 128:(i + 1) * 128, :], i, A1c, B1c, "modC", i, None)
        kc_t = alloc(2 * 512, F32).rearrange("p (c n) -> p c n", c=2)
        vc_t = alloc(2 * D, BF16).rearrange("p (c n) -> p c n", c=2)
        kscale = 128.0 ** -0.5
        for i in range(2):
            for (c0, nn, which) in ((0, 512, "k"), (512, 512, "v0"), (1024, 512, "v1")):
                bk = nb()
                op("pe", mmgroup([(ps[bk][:, :], hT[:, kc, i * 128:(i + 1) * 128], w_in[:, kc, c0:c0 + 512], kc == 0, kc == 7)
                                  for kc in range(8)]), r=["hT", "w_in"], w=[("ps", bk)])
                if which == "k":
                    tt("dve", kc_t[:, i, :], ps[bk][:, :], bbb[:, 0:512], ALU.add, r=[("ps", bk), "bbb"], w=["kc_t"])
                else:
                    vo = 0 if which == "v0" else 512
                    tt("dve", vc_t[:, i, vo:vo + 512], ps[bk][:, :], bbb[:, 512 + vo:512 + vo + 512], ALU.add,
                       r=[("ps", bk), "bbb"], w=["vc_t"])
        kcs = alloc(2 * 2 * 512, BF16).rearrange("p (d c n) -> p d c n", d=2, c=2)
        for i in range(2):
            for h in range(4):
                for d_ in range(2):
                    ts("dve", kcs[:, d_, i, h * 128:(h + 1) * 128], kc_t[:, i, h * 128:(h + 1) * 128],
                       DK[:, 4 * d_ + h:4 * d_ + h + 1], kscale, ALU.mult, ALU.mult, r=["kc_t", "DK"], w=["kcs"])
        s0v = s0.rearrange("p (d h n) -> p d h n", d=2, h=4)
        for h in range(4):
            for d_ in range(2):
                first, second = (0, 1) if d_ == 0 else (1, 0)
                bk = nb()
                op("pe", mmgroup([(ps[bk][:, 0:256], kcs[:, d_, first, h * 128:(h + 1) * 128], vc_t[:, first, h * 256:(h + 1) * 256], True, True),
                                  (ps[bk][:, 256:512], kcs[:, d_, second, h * 128:(h + 1) * 128], vc_t[:, second, h * 256:(h + 1) * 256], True, True)]),
                   r=["kcs", "vc_t"], w=[("ps", bk)])
                cp("act", tmpf[:, 0:256], ps[bk][:, 256:512], r=[("ps", bk)], w=["tmpf"])
                stt("dve", s0v[:, d_, h, :], ps[bk][:, 0:256], G128[:, 4 * d_ + h:4 * d_ + h + 1], tmpf[:, 0:256], ALU.mult, ALU.add,
                    r=[("ps", bk), "tmpf", "dec"], w=["s0"])

        phase_reset()
        A1 = alloc(D, F32)
        B1 = alloc(D, F32)
        dma("sp", B1, mod_d[:, 0:1024], w=["modL"])
        dma("sp", A1, mod_d[:, 1024:2048], w=["modL"])
        w_in = alloc(8 * INW, BF16).rearrange("p (k n) -> p k n", k=8)
        for kc in range(8):
            dma("pool", w_in[:, kc, :], w_in_v[:, kc, :], w=["w_in"])
        bfm = alloc(52, F32)
        dma("sp", bfm, b_in_fm_d, w=["bfm"])
        bbb = alloc(2560, F32)
        dma("sp", bbb, b_in_b_d, w=["bbb"])
        xt = [alloc(D, F32), alloc(D, F32)]
        hb4 = [alloc(D, BF16) for _ in range(4)]
        hT = alloc(8 * 512, BF16).rearrange("p (k t) -> p k t", k=8)
        ssq = alloc(4, F32)
        junk = alloc(D, BF16)
        ropet1 = alloc(1024, F32)
        ropet = [ropet1, ropet1]
        qkf = [alloc(512, F32) for _ in range(3)]
        qkr = [alloc(512, F32) for _ in range(3)]
        qko = [alloc(512, BF16) for _ in range(3)]
        ktm = alloc(4 * 512, BF16).rearrange("p (t n) -> p t n", t=4)
        outb = [alloc(512, BF16) for _ in range(3)]
        outf = [alloc(512, F32), alloc(512, F32)]
        sgm1 = alloc(512, F32)
        sgm = [sgm1, sgm1]
        permf = ca("permf")
        cnt = [0]
        bgjobs = []
        for e_ in range(32):
            bgjobs.append((ew1b_d[e_ * 128:(e_ + 1) * 128, :], ew1_d[e_ * 128:(e_ + 1) * 128, :]))
            bgjobs.append((ew3b_d[e_ * 128:(e_ + 1) * 128, :], ew3_d[e_ * 128:(e_ + 1) * 128, :]))
            bgjobs.append((ew2b_d[e_ * 512:(e_ + 1) * 512, :].rearrange("(p a) n -> p (a n)", p=128),
                           ew2_d[e_ * 512:(e_ + 1) * 512, :].rearrange("(p a) n -> p (a n)", p=128)))

        bgpos = [0]

        def bg_issue(n, rtags):
            for (o_, i_) in bgjobs[bgpos[0]:bgpos[0] + n]:
                S.add("pool", lambda e, o_=o_, i_=i_: e.dma_start(out=o_, in_=i_), r=rtags, dma=True, q="bg", bg=True)
            bgpos[0] += n

        def normA(g):
            for ti in range(4):
                i = g * 4 + ti
                b = i % 2
                r0 = g * 512 + ti * 128
                dma("sp", xt[b], x_d[r0:r0 + 128, :], w=[("xt", b)])
                sscol = ssq[:, b:b + 1]
                act(junk, xt[b], AF.Square, r=[("xt", b)], w=["junk", ("ssq", b)], accum=sscol)
                rstd_from_ss(sscol, D, ("ssq", b))
                stt("dve", xt[b], xt[b], sscol, A1, ALU.mult, ALU.mult, r=[("xt", b), ("ssq", b), "modL"], w=[("xt", b)])
                tt("dve", hb4[ti], xt[b], B1, ALU.add, r=[("xt", b), "modL"], w=[("hb4", ti)])

        def normB(g):
            for ti in range(4):
                bk = nb()
                op("pe", trgroup([(psb[bk][:, kc * 128:(kc + 1) * 128], hb4[ti][:, kc * 128:(kc + 1) * 128]) for kc in range(8)], identb),
                   r=[("hb4", ti), "cB"], w=[("ps", bk)])
                cp("act", hT[:, :, ti * 128:(ti + 1) * 128], psb[bk][:, :].rearrange("p (k t) -> p k t", k=8),
                   r=[("ps", bk)], w=["hT"])
        normA(0)
        normB(0)
        for g in range(8):
            t0 = g * 512
            rb = 0
            dma("sp", ropet[rb][:, 0:512], rope_d[:, g * 512:g * 512 + 512], w=[("ropet", rb)])
            dma("sp", ropet[rb][:, 512:1024], rope_d[:, L + g * 512:L + g * 512 + 512], w=[("ropet", rb)])
            bg_issue(5, ["hT"])
            if g + 1 < 8:
                normA(g + 1)

            def qk_mm(cc):
                s3 = cc % 3
                bk = nb()
                op("pe", mmgroup([(ps[bk][:, :], w_in[:, kc, cc * 128:(cc + 1) * 128], hT[:, kc, :], kc == 0, kc == 7)
                                  for kc in range(8)]), r=["hT", "w_in"], w=[("ps", bk)])
                act(qkf[s3], ps[bk][:, :], AF.Identity, r=[("ps", bk), "bfm"], w=[("qkf", s3)], bias=bfm[:, cc:cc + 1])

            def qk_rope(cc):
                s3 = cc % 3
                bk2 = nb()
                op("pe", mm(ps[bk2][:, :], permf, qkf[s3], True, True), r=[("qkf", s3), "cA"], w=[("ps", bk2)])
                tt("dve", qkr[s3], ps[bk2][:, :], ropet[rb][:, 512:1024], ALU.mult, r=[("ps", bk2), ("ropet", rb)], w=[("qkr", s3)])
                tt("dve", qkf[s3], qkf[s3], ropet[rb][:, 0:512], ALU.mult, r=[("qkf", s3), ("ropet", rb)], w=[("qkf", s3)])
                if cc < 4:
                    tt("dve", qko[s3], qkf[s3], qkr[s3], ALU.add, r=[("qkf", s3), ("qkr", s3)], w=[("qko", s3)])
                    dma("sp", qT_d[cc, :, t0:t0 + 512], qko[s3], r=[("qko", s3)])
                else:
                    h = cc - 4
                    tt("dve", qkr[s3], qkf[s3], qkr[s3], ALU.add, r=[("qkf", s3), ("qkr", s3)], w=[("qkr", s3)])
                    act(qko[s3], qkr[s3], AF.Copy, r=[("qkr", s3)], w=[("qko", s3)], scale=kscale)
                    dma("sp", kT_d[h, :, t0:t0 + 512], qko[s3], r=[("qko", s3)])
                    bk3 = nb()
                    op("pe", trgroup([(psb[bk3][:, ti * 128:(ti + 1) * 128], qko[s3][:, ti * 128:(ti + 1) * 128]) for ti in range(4)], identb),
                       r=[("qko", s3), "cB"], w=[("ps", bk3)])
                    cp("act", ktm[:, :, h * 128:(h + 1) * 128], psb[bk3][:, 0:512].rearrange("p (t n) -> p t n", t=4),
                       r=[("ps", bk3)], w=["ktm"])
            for cc in range(8):
                qk_mm(cc)
                if cc >= 1:
                    qk_rope(cc - 1)
            pend = [7]
            for ti in range(4):
                for nh in range(4):
                    c0 = 1024 + nh * 512
                    bk = nb()
                    op("pe", mmgroup([(ps[bk][:, :], hT[:, kc, ti * 128:(ti + 1) * 128], w_in[:, kc, c0:c0 + 512], kc == 0, kc == 7)
                                      for kc in range(8)]), r=["hT", "w_in"], w=[("ps", bk)])
                    if pend:
                        qk_rope(pend.pop())
                        dma("sp", kt_d[t0:t0 + 512, :].rearrange("(t p) n -> p t n", p=128), ktm, r=["ktm"])
                    o3 = cnt[0] % 3
                    ob = outb[o3]
                    otag = ("outb", o3)
                    cnt[0] += 1
                    r0 = t0 + ti * 128
                    if nh < 2:
                        tt("dve", ob, ps[bk][:, :], bbb[:, 512 + nh * 512:1024 + nh * 512], ALU.add, r=[("ps", bk), "bbb"], w=[otag])
                        dma("sp", v_d[r0:r0 + 128, nh * 512:(nh + 1) * 512], ob, r=[otag])
                    else:
                        f2 = nh % 2
                        tt("dve", outf[f2], ps[bk][:, :], bbb[:, 512 + nh * 512:1024 + nh * 512], ALU.add, r=[("ps", bk), "bbb"], w=[("outf", f2)])
                        act(sgm[f2], outf[f2], AF.Sigmoid, r=[("outf", f2)], w=[("sgm", 0)])
                        tt("dve", ob, outf[f2], sgm[f2], ALU.mult, r=[("outf", f2), ("sgm", 0)], w=[otag])
                        dma("sp", gs_d[r0:r0 + 128, (nh - 2) * 512:(nh - 1) * 512], ob, r=[otag])
            for cc in range(28):
                c0 = 3072 + cc * 128
                bk = nb()
                op("pe", mmgroup([(ps[bk][:, :], w_in[:, kc, c0:c0 + 128], hT[:, kc, :], kc == 0, kc == 7)
                                  for kc in range(8)]), r=["hT", "w_in"], w=[("ps", bk)])
                o3 = cnt[0] % 3
                ob = outb[o3]
                otag = ("outb", o3)
                cnt[0] += 1
                act(ob, ps[bk][:, :], AF.Identity if cc < 12 else AF.Sigmoid, r=[("ps", bk), "bfm"], w=[otag],
                    bias=bfm[:, 24 + cc:25 + cc])
                if cc < 12:
                    dma("sp", u_d[cc * 128:(cc + 1) * 128, t0:t0 + 512], ob, r=[otag])
                else:
                    dma("sp", gates_d[(cc - 12) * 128:(cc - 11) * 128, t0:t0 + 512], ob, r=[otag])
            if g + 1 < 8:
                normB(g + 1)

        phase_reset()
        qTt2 = [alloc(L, BF16) for _ in range(2)]
        kTt2 = [alloc(L, BF16) for _ in range(2)]
        qfb2 = [alloc(2 * L, BF16).rearrange("p (d t) -> p d t", d=2) for _ in range(2)]
        ktk1 = alloc(32 * 128, BF16).rearrange("p (c n) -> p c n", c=32)
        kfb2 = [alloc(2 * 32 * 128, BF16).rearrange("p (d c n) -> p d c n", d=2, c=32) for _ in range(2)]
        vt2 = [alloc(32 * 256, BF16).rearrange("p (c n) -> p c n", c=32) for _ in range(2)]
        SB = alloc(32 * 256, BF16).rearrange("p (c n) -> p c n", c=32)
        SF = alloc(32 * 256, BF16).rearrange("p (c n) -> p c n", c=32)
        SstF = [alloc(256, F32), alloc(256, F32)]
        SstB = [alloc(256, F32), alloc(256, F32)]
        PT = [alloc(128, BF16) for _ in range(3)]
        gst = [alloc(256, BF16) for _ in range(3)]
        rgo = [alloc(256, BF16) for _ in range(3)]
        rss = alloc(4, F32)
        junk = alloc(256, BF16)
        def loadpre(h):
            hb_ = h % 2
            qTt, kTt, qfb, kfb, vt, ktk = qTt2[hb_], kTt2[hb_], qfb2[hb_], kfb2[hb_], vt2[hb_], ktk1
            dma("sp", qTt, qT_d[h], w=[("qTt", hb_)])
            dma("sp", kTt, kT_d[h], w=[("kTt", hb_)])
            dma("sp", ktk, kt_d[:, h * 128:(h + 1) * 128].rearrange("(c p) n -> p c n", p=128), w=["ktk"])
            dma("sp", vt, v_d[:, h * 256:(h + 1) * 256].rearrange("(c p) n -> p c n", p=128), w=[("vt", hb_)])
            for d_ in range(2):
                tt("dve", qfb[:, d_, :].rearrange("p (c i) -> p c i", i=128), qTt.rearrange("p (c i) -> p c i", i=128),
                   DQf[:, 4 * d_ + h, :].unsqueeze(1).to_broadcast([128, 32, 128]), ALU.mult, r=[("qTt", hb_), "DQ"], w=[("qfb", hb_, d_)])
                act(kfb[:, d_].rearrange("p c n -> p (c n)"), ktk.rearrange("p c n -> p (c n)"), AF.Copy, r=["ktk", "DK"], w=[("kfb", hb_, d_)],
                    scale=DK[:, 4 * d_ + h:4 * d_ + h + 1])
        loadpre(0)
        for h in range(4):
            hb_ = h % 2
            qTt, kTt, qfb, kfb, vt = qTt2[hb_], kTt2[hb_], qfb2[hb_], kfb2[hb_], vt2[hb_]
            cp("dve", SstF[0], s0v[:, 0, h, :], r=["s0"], w=[("SstF", 0)])
            cp("act", SF[:, 0, :], s0v[:, 0, h, :], r=["s0"], w=[("SF", 0)])
            cp("dve", SstB[0], s0v[:, 1, h, :], r=["s0"], w=[("SstB", 0)])
            cp("act", SB[:, 31, :], s0v[:, 1, h, :], r=["s0"], w=[("SB", 31)])
            for i in range(31):
                cur, nxt = i % 2, (i + 1) % 2
                cf, cbk = i, 31 - i
                bk = nb()
                op("pe", mmgroup([(ps[bk][:, 0:256], kfb[:, 0, cf, :], vt[:, cf, :], True, True),
                                  (ps[bk][:, 256:512], kfb[:, 1, cbk, :], vt[:, cbk, :], True, True)]),
                   r=[("kfb", hb_, 0), ("kfb", hb_, 1), ("vt", hb_)], w=[("ps", bk)])
                stt("dve", SstF[nxt], SstF[cur], G128[:, h:h + 1], ps[bk][:, 0:256], ALU.mult, ALU.add,
                    r=[("SstF", cur), ("ps", bk), "dec"], w=[("SstF", nxt)])
                stt("dve", SstB[nxt], SstB[cur], G128[:, 4 + h:5 + h], ps[bk][:, 256:512], ALU.mult, ALU.add,
                    r=[("SstB", cur), ("ps", bk), "dec"], w=[("SstB", nxt)])
                cp("act", SF[:, cf + 1, :], SstF[nxt], r=[("SstF", nxt)], w=[("SF", cf + 1)])
                cp("act", SB[:, cbk - 1, :], SstB[nxt], r=[("SstB", nxt)], w=[("SB", cbk - 1)])

            if h + 1 < 4:
                loadpre(h + 1)

            def scores(c):
                b3 = c % 3
                bk = nb()
                op("pe", mm(ps[bk][:, 0:128], kTt[:, c * 128:(c + 1) * 128], qTt[:, c * 128:(c + 1) * 128], True, True),
                   r=[("kTt", hb_), ("qTt", hb_)], w=[("ps", bk)])
                tt("dve", PT[b3], ps[bk][:, 0:128], Dm[:, h, :], ALU.mult, r=[("ps", bk), "Dm"], w=[("PT", b3)])
                dma("sp", gst[b3], gs_d[c * 128:(c + 1) * 128, h * 256:(h + 1) * 256], w=[("gst", b3)])

            def outc(c):
                b3 = c % 3
                bo = nb()
                op("pe", mmgroup([(ps[bo][:, 0:256], PT[b3], vt[:, c, :], True, False),
                                  (ps[bo][:, 0:256], qfb[:, 0, c * 128:(c + 1) * 128], SF[:, c, :], False, False),
                                  (ps[bo][:, 0:256], qfb[:, 1, c * 128:(c + 1) * 128], SB[:, c, :], False, True)]),
                   r=[("PT", b3), ("vt", hb_), ("qfb", hb_, 0), ("qfb", hb_, 1), ("SF", c), ("SB", c)], w=[("ps", bo)])
                sscol = rss[:, b3:b3 + 1]
                act(junk, ps[bo][:, 0:256], AF.Square, r=[("ps", bo)], w=["junk", ("rss", b3)], accum=sscol)
                rstd_from_ss(sscol, 256, ("rss", b3))
                stt("dve", rgo[b3], ps[bo][:, 0:256], sscol, gst[b3], ALU.mult, ALU.mult,
                    r=[("ps", bo), ("rss", b3), ("gst", b3)], w=[("rgo", b3)])
                dma("sp", rg_d[c * 128:(c + 1) * 128, h * 256:(h + 1) * 256], rgo[b3], r=[("rgo", b3)])
                if c % 3 == 0 and c < 30:
                    bg_issue(1, [("rgo", b3)])
            scores(0)
            scores(1)
            for c in range(32):
                if c + 2 < 32:
                    scores(c + 2)
                outc(c)

        phase_reset()
        fT = alloc(L, F32)
        dma("sp", fT[0:33, :], featsT_d, w=["fT"])
        hw1 = alloc(64, F32)
        dma("sp", hw1[0:33, :], hw1_d, w=["hw1"])
        hvec = alloc(4, F32)
        dma("sp", hvec[0:64, :], hvec_d, w=["hvec"])
        hw2 = alloc(64, F32)
        dma("sp", hw2[0:64, :], hw2_d, w=["hw2"])
        hw3 = alloc(1024, F32)
        dma("sp", hw3[0:64, :], hw3_d, w=["hw3"])
        tv = alloc(L, F32)
        dma("sp", tv, tvec_d, w=["tv"])
        fb = alloc(2, F32)
        tt("dve", fb[0:64, 0:1], hvec[0:64, 0:1], hvec[0:64, 1:2], ALU.mult, r=["hvec"], w=["fb"])
        tt("dve", fb[0:64, 1:2], hvec[0:64, 2:3], hvec[0:64, 1:2], ALU.mult, r=["hvec"], w=["fb"])
        hid = [alloc(L, F32), alloc(L, F32)]
        argt = alloc(512, F32)
        kti = alloc(512, I32)
        ktf = alloc(512, F32)
        TWO_PI = 2.0 * math.pi

        def sin_layer(dst, bank, bcol):
            act(argt[0:64, :], ps[bank][0:64, :], AF.Identity, r=[("ps", bank), "hvec", "fb"], w=["argt"],
                scale=hvec[0:64, 1:2], bias=fb[0:64, bcol:bcol + 1])
            ts("dve", ktf[0:64, :], argt[0:64, :], 1.0 / TWO_PI, None, ALU.mult, None, r=["argt"], w=["ktf"])
            cp("dve", kti[0:64, :], ktf[0:64, :], r=["ktf"], w=["kti"])
            cp("dve", ktf[0:64, :], kti[0:64, :], r=["kti"], w=["ktf"])
            stt("dve", argt[0:64, :], ktf[0:64, :], -TWO_PI, argt[0:64, :], ALU.mult, ALU.add, r=["ktf", "argt"], w=["argt"])
            act(dst, argt[0:64, :], AF.Sin, r=["argt"], w=["hid"])
        for tg in range(8):
            sl = slice(tg * 512, (tg + 1) * 512)
            bk = nb()
            op("pe", mm(ps[bk][0:64, :], hw1[0:33, :], fT[0:33, sl], True, True), r=["hw1", "fT"], w=[("ps", bk)])
            sin_layer(hid[0][0:64, sl], bk, 0)
        for tg in range(8):
            sl = slice(tg * 512, (tg + 1) * 512)
            bk = nb()
            op("pe", mm(ps[bk][0:64, :], hw2[0:64, :], hid[0][0:64, sl], True, True), r=["hw2", "hid"], w=[("ps", bk)])
            sin_layer(hid[1][0:64, sl], bk, 1)
        S.barrier()
        fl = [alloc(L, F32), alloc(L, F32)]
        l1 = alloc(8, F32)
        win = alloc(512, F32)
        nd = ca("negdelta")
        win = [win, alloc(512, F32)]
        hsb = [alloc(L, BF16), alloc(L, BF16)]
        for cp_ in range(4):
            for b2 in range(2):
                cc = cp_ + 4 * b2
                for tg in range(8):
                    sl = slice(tg * 512, (tg + 1) * 512)
                    bk = nb()
                    w2_ = tg % 2
                    op("pe", mm(ps[bk][:, :], hw3[0:64, cc * 128:(cc + 1) * 128], hid[1][0:64, sl], True, True), r=["hw3"], w=[("ps", bk)])
                    act(win[w2_], tv[:, sl], AF.Exp, r=["tv", "cA"], w=[("win", w2_)], scale=nd[:, cc:cc + 1])
                    tt("dve", fl[b2][:, sl], ps[bk][:, :], win[w2_], ALU.mult, r=[("ps", bk), ("win", w2_)], w=[("fl", b2)])
                op("dve", lambda e, b2=b2, cc=cc: e.tensor_reduce(out=l1[:, cc:cc + 1], in_=fl[b2], axis=AX.X, op=ALU.add, apply_absolute_value=True),
                   r=[("fl", b2)], w=["l1"])
                op("dve", lambda e, cc=cc: e.reciprocal(out=l1[:, cc:cc + 1], in_=l1[:, cc:cc + 1]), r=["l1"], w=["l1"])
            act(fl[1], fl[1], AF.Copy, r=[("fl", 1), "l1"], w=[("fl", 1)], scale=l1[:, cp_ + 4:cp_ + 5])
            stt("dve", hsb[0], fl[0], l1[:, cp_:cp_ + 1], fl[1], ALU.mult, ALU.add, r=[("fl", 0), ("fl", 1), "l1"], w=[("hsb", 0)])
            stt("dve", hsb[1], fl[0], l1[:, cp_:cp_ + 1], fl[1], ALU.mult, ALU.subtract, r=[("fl", 0), ("fl", 1), "l1"], w=[("hsb", 1)])
            dma("sp", hs_d[0, cp_ * 128:(cp_ + 1) * 128, :], hsb[0], r=[("hsb", 0)], w=["hs_d"])
            dma("sp", hs_d[1, cp_ * 128:(cp_ + 1) * 128, :], hsb[1], r=[("hsb", 1)], w=["hs_d"])

        E1c, E1s, E1cT, E1sT = cb("E1c"), cb("E1s"), cb("E1cT"), cb("E1sT")
        cdv = cd_d.rearrange("r (k a c) -> r k a c", k=128, a=32)
        cdv2 = cd_d.rearrange("r (q m a c) -> r m a q c", q=32, m=4, a=32)
        FB = {}

        def fft_forward(want, consume):
            sigF, sigJ, C2, ftab, stg = FB["sigF"], FB["sigJ"], FB["C2"], FB["ftab"], FB["stg"]
            sv = sigF.rearrange("p k (j a) -> p k a j", a=32)
            for a2 in range(16):
                bk = nb()
                op("pe", trgroup([(psb[bk][:, (aa * 4 + cc) * 128:(aa * 4 + cc + 1) * 128], sv[:, cc, a2 * 2 + aa, :])
                                  for aa in range(2) for cc in range(4)], identb), r=["sigF", "cB"], w=[("ps", bk)])
                cp("act" if a2 % 2 == 0 else "dve", sigJ[:, a2 * 2:a2 * 2 + 2, :], psb[bk][:, :].rearrange("p (a c) -> p a c", a=2),
                   r=[("ps", bk)], w=["sigJ"])
            for a in range(32):
                for ri in range(2):
                    bk = nb()
                    op("pe", mm(ps[bk][:, :], E1c if ri == 0 else E1s, sigJ[:, a, :], True, True), r=["sigJ", "cB"], w=[("ps", bk)])
                    cp("act" if ri == 0 else "dve", C2[ri][:, a, :], ps[bk][:, :], r=[("ps", bk)], w=[("C2", ri)])
            for ri in range(2):
                dma("sp", cdv[ri], C2[ri], r=[("C2", ri)], w=[("cd", ri)])
            for ri in range(2):
                for m in range(4):
                    dma("sp", C2[ri][m * 32:(m + 1) * 32, :, :], cdv2[ri, m], r=[("cd", ri)], w=[("C2", ri)])
            for q in range(32):
                bre = bim = None
                if want in ("re", "both"):
                    bre = nb()
                    op("pe", mmgroup([(ps[bre][:, :], ftab[:, 0, q, :], C2[0][:, q, :], True, False),
                                      (ps[bre][:, :], ftab[:, 2, q, :], C2[1][:, q, :], False, True)]),
                       r=["ftab", ("C2", 0), ("C2", 1)], w=[("ps", bre)])
                if want in ("im", "both"):
                    bim = nb()
                    op("pe", mmgroup([(ps[bim][:, :], ftab[:, 1, q, :], C2[0][:, q, :], True, False),
                                      (ps[bim][:, :], ftab[:, 0, q, :], C2[1][:, q, :], False, True)]),
                       r=["ftab", ("C2", 0), ("C2", 1)], w=[("ps", bim)])
                consume(q, bre, bim)

        phase_reset()
        C2 = [alloc(32 * 512, BF16).rearrange("p (q c) -> p q c", q=32) for _ in range(2)]
        ftab = alloc(3 * 32 * 128, BF16).rearrange("p (y q m) -> p y q m", y=3, q=32)
        dma("sp", ftab, ftab_d.rearrange("p (y q m) -> p y q m", y=3, q=32), w=["ftab"])
        sigF = alloc(4 * L, BF16).rearrange("p (k t) -> p k t", k=4)
        sigJ = alloc(32 * 512, BF16).rearrange("p (a c) -> p a c", a=32)
        FB.update(sigF=sigF, sigJ=sigJ, C2=C2, ftab=ftab, stg=None)
        hstage = [alloc(2048, F32), alloc(2048, F32)]
        for which in range(2):
            dma("sp", sigF, hs_d[which].rearrange("(k p) t -> p k t", p=128), r=["hs_d"], w=["sigF"])

            def consume_h(q, bre, bim, which=which):
                bk = bre if which == 0 else bim
                hi_ = (q // 4) % 2
                q4 = q % 4
                hs = hstage[hi_]
                tg_ = ("hstage", hi_)
                cp("act", hs[:, q4 * 512:(q4 + 1) * 512], ps[bk][:, :], r=[("ps", bk)], w=[tg_])
                if q4 == 3:
                    dma("sp", hd_d[which, :, (q - 3) * 512:(q + 1) * 512], hs, r=[tg_], w=["hd"])
                    bg_issue(1, [tg_])
            fft_forward("re" if which == 0 else "im", consume_h)

        phase_reset()
        C2 = [alloc(32 * 512, BF16).rearrange("p (q c) -> p q c", q=32) for _ in range(2)]
        mark0 = st["off"]
        ftab = alloc(3 * 32 * 128, BF16).rearrange("p (y q m) -> p y q m", y=3, q=32)
        dma("sp", ftab, ftab_d.rearrange("p (y q m) -> p y q m", y=3, q=32), w=["ftab"])
        sigF = alloc(4 * L, BF16).rearrange("p (k t) -> p k t", k=4)
        hyc = alloc(60, F32).rearrange("p (c k) -> p c k", c=12)
        dma("sp", hyc, hyc_d.rearrange("p (c k) -> p c k", c=12), w=["hyc"])
        hsk = alloc(4, F32)
        dma("sp", hsk, hyskip_d, w=["hsk"])
        mark1 = st["off"]
        ub = alloc(L + 2, BF16)
        cacc = alloc(L, F32)
        x1c = alloc(L, F32)

        def conv_chunk(cc, dst, ub, cacc):
            dma("sp", ub[:, 1:L + 1], u_d[cc * 128:(cc + 1) * 128, :], w=["ub"])
            ts("dve", cacc, ub[:, 0:L], hyc[:, cc, 0:1], hyc[:, cc, 3:4], ALU.mult, ALU.add, r=["ub", "hyc"], w=["cacc"])
            stt("dve", cacc, ub[:, 1:L + 1], hyc[:, cc, 1:2], cacc, ALU.mult, ALU.add, r=["ub", "hyc", "cacc"], w=["cacc"])
            stt("dve", dst, ub[:, 2:L + 2], hyc[:, cc, 2:3], cacc, ALU.mult, ALU.add, r=["ub", "hyc", "cacc"], w=["convout"])
        op("dve", lambda e: e.memset(ub, 0.0), w=["ub"])
        for cc in range(4):
            conv_chunk(4 + cc, x1c, ub, cacc)
            conv_chunk(8 + cc, cacc, ub, cacc)
            tt("dve", sigF[:, cc, :], cacc, x1c, ALU.mult, r=["convout"], w=["sigF", "convout"])
            dma("sp", z_d[cc * 128:(cc + 1) * 128, :], sigF[:, cc, :], r=["sigF"], w=["z_d"])
        S.barrier()
        st["off"] = mark1
        sigJ = alloc(32 * 512, BF16).rearrange("p (a c) -> p a c", a=32)
        FB.update(sigF=sigF, sigJ=sigJ, C2=C2, ftab=ftab, stg=None)
        Yst = [sigJ, sigF.rearrange("p k t -> p (k t)").rearrange("p (q c) -> p q c", q=32)]
        ytag = ["sigJ", "sigF"]
        hre = [alloc(512, F32), alloc(512, F32)]
        him = [alloc(512, F32), alloc(512, F32)]
        xre2 = [alloc(512, F32), alloc(512, F32)]
        xim2 = [alloc(512, F32), alloc(512, F32)]
        t12 = [alloc(512, F32), alloc(512, F32)]
        t22 = [alloc(512, F32), alloc(512, F32)]

        def consume_z(q, bre, bim):
            b2 = q % 2
            xre, xim, t1, t2 = xre2[b2], xim2[b2], t12[b2], t22[b2]
            dma("sp", hre[b2], hd_d[0, :, q * 512:(q + 1) * 512], w=[("hre", b2)])
            dma("sp", him[b2], hd_d[1, :, q * 512:(q + 1) * 512], w=[("him", b2)])
            cp("act", xre, ps[bre][:, :], r=[("ps", bre)], w=[("xre", b2)])
            cp("act", xim, ps[bim][:, :], r=[("ps", bim)], w=[("xim", b2)])
            tt("dve", t1, xre, hre[b2], ALU.mult, r=[("xre", b2), ("hre", b2)], w=[("t1", b2)])
            tt("dve", t2, xim, him[b2], ALU.mult, r=[("xim", b2), ("him", b2)], w=[("t2", b2)])
            tt("dve", Yst[0][:, q, :], t1, t2, ALU.subtract, r=[("t1", b2), ("t2", b2)], w=[ytag[0]])
            tt("dve", t1, xre, him[b2], ALU.mult, r=[("xre", b2), ("him", b2)], w=[("t1", b2)])
            tt("dve", t2, xim, hre[b2], ALU.mult, r=[("xim", b2), ("hre", b2)], w=[("t2", b2)])
            tt("dve", Yst[1][:, q, :], t1, t2, ALU.add, r=[("t1", b2), ("t2", b2)], w=[ytag[1]])
        fft_forward("both", consume_z)
        itab = ftab
        dma("sp", itab, itab_d.rearrange("p (y q m) -> p y q m", y=3, q=32), w=["ftab"])
        for q in range(32):
            for ri in range(2):
                bk = nb()
                if ri == 0:
                    lst = [(ps[bk][:, :], itab[:, 0, q, :], Yst[0][:, q, :], True, False), (ps[bk][:, :], itab[:, 1, q, :], Yst[1][:, q, :], False, True)]
                else:
                    lst = [(ps[bk][:, :], itab[:, 0, q, :], Yst[1][:, q, :], True, False), (ps[bk][:, :], itab[:, 2, q, :], Yst[0][:, q, :], False, True)]
                op("pe", mmgroup(lst), r=["ftab", ytag[0], ytag[1]], w=[("ps", bk)])
                cp("act" if ri == 0 else "dve", C2[ri][:, q, :], ps[bk][:, :], r=[("ps", bk)], w=[("C2", ri)])
        Dst = [c_.rearrange("p q c -> p (q c)").rearrange("p (a c) -> p a c", a=32) for c_ in C2]
        for ri in range(2):
            for m in range(4):
                dma("sp", cdv2[ri, m], C2[ri][m * 32:(m + 1) * 32, :, :], r=[("C2", ri)], w=[("cd", ri)])
        for ri in range(2):
            dma("sp", Dst[ri], cdv[ri], r=[("cd", ri)], w=[("C2", ri)])
        S.barrier()
        st["off"] = mark0
        ub = alloc(L + 2, BF16)
        cacc = alloc(L, F32)
        hyo = alloc(L, BF16)
        ycv = alloc(L, F32)
        zr = alloc(L, BF16)
        hyc = alloc(60, F32).rearrange("p (c k) -> p c k", c=12)
        dma("sp", hyc, hyc_d.rearrange("p (c k) -> p c k", c=12), w=["hyc"])
        hsk = alloc(4, F32)
        dma("sp", hsk, hyskip_d, w=["hsk"])
        op("dve", lambda e: e.memset(ub, 0.0), w=["ub"])
        for cc in range(4):
            yv = ycv.rearrange("p (j a) -> p a j", a=32)
            for a4 in range(8):
                bk = nb()
                lst = []
                for aa in range(4):
                    a = a4 * 4 + aa
                    lst.append((ps[bk][:, aa * 128:(aa + 1) * 128], Dst[0][:, a, cc * 128:(cc + 1) * 128], E1cT, True, False))
                    lst.append((ps[bk][:, aa * 128:(aa + 1) * 128], Dst[1][:, a, cc * 128:(cc + 1) * 128], E1sT, False, True))
                op("pe", mmgroup(lst), r=[("C2", 0), ("C2", 1), "cB"], w=[("ps", bk)])
                act(yv[:, a4 * 4:a4 * 4 + 4, :], ps[bk][:, :].rearrange("p (a j) -> p a j", a=4), AF.Copy,
                    r=[("ps", bk)], w=["ycv"], scale=1.0 / 4096.0)
            dma("sp", zr, z_d[cc * 128:(cc + 1) * 128, :], w=["zr"])
            stt("dve", ycv, zr, hsk[:, cc:cc + 1], ycv, ALU.mult, ALU.add, r=["zr", "hsk", "ycv"], w=["ycv"])
            conv_chunk(cc, cacc, ub, cacc)
            tt("dve", hyo, ycv, cacc, ALU.mult, r=["ycv", "convout"], w=["hyo", "convout"])
            dma("sp", hy_d[cc * 128:(cc + 1) * 128, :], hyo, r=["hyo"])

        phase_reset()
        h2all = alloc(NT * D, BF16).rearrange("p (t n) -> p t n", t=NT)
        oh_all = alloc(NT * 64, BF16).rearrange("p (t k e) -> p t k e", t=NT, k=2)
        rk_all = alloc(NT * 2, F32).rearrange("p (t k) -> p t k", t=NT)
        wtsv = wts.rearrange("p (t k) -> p t k", t=NT)
        destv = dest.rearrange("p (t k) -> p t k", t=NT)
        base = alloc(32, F32)
        op("dve", lambda e: e.memset(base, 0.0), w=["base"])
        mark = st["off"]
        rwo = alloc(8 * D, BF16).rearrange("p (k n) -> p k n", k=8)
        hwo = alloc(4 * D, BF16).rearrange("p (k n) -> p k n", k=4)
        wo = alloc(8 * D, BF16).rearrange("p (k n) -> p k n", k=8)
        dma("pool", rwo, ret_w_o_d.rearrange("(k p) n -> p k n", p=128), w=["rwo"])
        dma("pool", hwo, hy_w_o_d.rearrange("(k p) n -> p k n", p=128), w=["hwo"])
        dma("pool", wo, w_out_d.rearrange("(k p) n -> p k n", p=128), w=["wo"])
        wr = alloc(8 * 36, F32).rearrange("p (k n) -> p k n", k=8)
        dma("sp", wr, wr_d.rearrange("(k p) n -> p k n", p=128), w=["wr"])
        brb = alloc(36, F32)
        dma("sp", brb, br_d, w=["brb"])
        rgt = [alloc(D, BF16), alloc(D, BF16)]
        rgT = alloc(8 * 512, BF16).rearrange("p (k t) -> p k t", k=8)
        hyT = alloc(4 * 512, BF16).rearrange("p (k t) -> p k t", k=4)
        gt_ = [alloc(512, BF16), alloc(512, BF16)]
        gh_ = [alloc(512, BF16), alloc(512, BF16)]
        m1 = alloc(512, F32)
        m2 = alloc(512, F32)
        mixT = alloc(8 * 512, BF16).rearrange("p (k t) -> p k t", k=8)
        x2t = [alloc(D, F32), alloc(D, F32)]
        GT1 = alloc(D, F32)
        A2 = alloc(D, F32)
        B2 = alloc(D, F32)
        dma("sp", GT1, mod_d[:, 2048:3072], w=["modL"])
        dma("sp", B2, mod_d[:, 3072:4096], w=["modL"])
        dma("sp", A2, mod_d[:, 4096:5120], w=["modL"])
        h2f = alloc(D, F32)
        h2T = alloc(8 * 128, F32).rearrange("p (k t) -> p k t", k=8)
        ss2 = alloc(2, F32)
        lg = alloc(36, F32)
        sm = alloc(64, F32)
        elm = alloc(32, F32)
        top8 = alloc(8, F32)
        ohb = alloc(32, BF16)
        posn = alloc(32, F32)
        junk2 = alloc(D, BF16)
        tri = cb("tri")
        onesb = cb("onesb")
        h2f2 = [h2f, alloc(D, F32)]
        h2T2 = [h2T, alloc(8 * 128, F32).rearrange("p (k t) -> p k t", k=8)]
        ohb2 = [ohb, alloc(32, BF16)]
        sm4 = alloc(32, F32)

        def T1(tile_i, ti):
            b = tile_i % 2
            r0 = tile_i * 128
            hf_ = h2f2[b]
            dma("sp", x2t[b], x_d[r0:r0 + 128, :], w=[("x2t", b)])
            for nh in range(2):
                bk = nb()
                op("pe", mmgroup([(ps[bk][:, :], mixT[:, kc, ti * 128:(ti + 1) * 128], wo[:, kc, nh * 512:(nh + 1) * 512], kc == 0, kc == 7)
                                  for kc in range(8)]), r=["mixT", "wo"], w=[("ps", bk)])
                sl = slice(nh * 512, (nh + 1) * 512)
                tt("dve", m1, ps[bk][:, :], GT1[:, sl], ALU.mult, r=[("ps", bk), "modL"], w=["m1"])
                tt("dve", x2t[b][:, sl], x2t[b][:, sl], m1, ALU.add, r=[("x2t", b), "m1"], w=[("x2t", b)])
            dma("sp", x2_d[r0:r0 + 128, :], x2t[b], r=[("x2t", b)])
            sscol = ss2[:, b:b + 1]
            act(junk2, x2t[b], AF.Square, r=[("x2t", b)], w=["junk2", ("ss2", b)], accum=sscol)
            rstd_from_ss(sscol, D, ("ss2", b))
            stt("dve", hf_, x2t[b], sscol, A2, ALU.mult, ALU.mult, r=[("x2t", b), ("ss2", b), "modL"], w=[("h2f", b)])
            tt("dve", hf_, hf_, B2, ALU.add, r=[("h2f", b), "modL"], w=[("h2f", b)])
            cp("act", h2all[:, tile_i, :], hf_, r=[("h2f", b)], w=[("h2all", tile_i)])

        def T2(tile_i):
            b = tile_i % 2
            hf_ = h2f2[b]
            hT_ = h2T2[b]
            bk0, bk1 = nb(), nb()
            op("pe", trgroup([((ps[bk0] if kc < 4 else ps[bk1])[:, (kc % 4) * 128:(kc % 4 + 1) * 128], hf_[:, kc * 128:(kc + 1) * 128])
                              for kc in range(8)], identf), r=[("h2f", b), "cA"], w=[("ps", bk0), ("ps", bk1)])
            cp("act", hT_[:, 0:4, :], ps[bk0][:, :].rearrange("p (k t) -> p k t", k=4), r=[("ps", bk0)], w=[("h2T", b)])
            cp("act", hT_[:, 4:8, :], ps[bk1][:, :].rearrange("p (k t) -> p k t", k=4), r=[("ps", bk1)], w=[("h2T", b)])

        def T3(tile_i):
            b = tile_i % 2
            hT_ = h2T2[b]
            ohb_ = ohb2[b]
            bl = nb()
            op("pe", mmgroup([(ps[bl][:, 0:36], hT_[:, kc, :], wr[:, kc, :], kc == 0, kc == 7) for kc in range(8)]),
               r=[("h2T", b), "wr"], w=[("ps", bl)])
            tt("dve", lg, ps[bl][:, 0:36], brb, ALU.add, r=[("ps", bl), "brb"], w=["lg"])
            gmax = sm[:, 0:1]
            op("dve", lambda e: e.tensor_reduce(out=sm[:, 0:1], in_=lg[:, 0:4], axis=AX.X, op=ALU.max), r=["lg"], w=["sm"])
            ts("dve", sm[:, 4:8], lg[:, 0:4], gmax, None, ALU.subtract, None, r=["lg", "sm"], w=["sm"])
            act(sm[:, 8:12], sm[:, 4:8], AF.Exp, r=["sm"], w=["sm"], accum=sm[:, 1:2])
            op("dve", lambda e: e.reciprocal(out=sm[:, 2:3], in_=sm[:, 1:2]), r=["sm"], w=["sm"])
            ts("dve", sm[:, 12:16], lg[:, 0:4], gmax, None, ALU.is_equal, None, r=["lg", "sm"], w=["sm"])
            ts("dve", sm[:, 16:20], sm[:, 12:16], 1e30, -1e30, ALU.mult, ALU.add, r=["sm"], w=["sm"])
            tt("dve", elm.rearrange("p (g e) -> p g e", g=4), lg[:, 4:36].rearrange("p (g e) -> p g e", g=4),
               sm[:, 12:16].unsqueeze(2).to_broadcast([128, 4, 8]), ALU.mult, r=["lg", "sm"], w=["elm"])
            tt("dve", elm.rearrange("p (g e) -> p g e", g=4), elm.rearrange("p (g e) -> p g e", g=4),
               sm[:, 16:20].unsqueeze(2).to_broadcast([128, 4, 8]), ALU.add, r=["elm", "sm"], w=["elm"])
            op("dve", lambda e: e.max(out=top8, in_=elm), r=["elm"], w=["top8"])
            tt("dve", sm[:, 20:21], top8[:, 1:2], top8[:, 0:1], ALU.subtract, r=["top8"], w=["sm"])
            act(sm[:, 21:22], sm[:, 20:21], AF.Exp, r=["sm"], w=["sm"])
            ts("dve", sm[:, 21:22], sm[:, 21:22], 1.0, None, ALU.add, None, r=["sm"], w=["sm"])
            op("dve", lambda e: e.reciprocal(out=sm[:, 22:23], in_=sm[:, 21:22]), r=["sm"], w=["sm"])
            ts("dve", sm[:, 23:24], sm[:, 22:23], -1.0, 1.0, ALU.mult, ALU.add, r=["sm"], w=["sm"])
            tt("dve", wtsv[:, tile_i, :], sm[:, 22:24], sm[:, 2:3].to_broadcast([128, 2]), ALU.mult, r=["sm"], w=["wts"])
            for k_ in range(2):
                ts("dve", oh_all[:, tile_i, k_, :], elm, top8[:, k_:k_ + 1], None, ALU.is_equal, None, r=["elm", "top8"], w=[("oh", tile_i)])
            tt("dve", ohb_, oh_all[:, tile_i, 0, :], oh_all[:, tile_i, 1, :], ALU.add, r=[("oh", tile_i)], w=[("ohb", b)])

        def T4(tile_i):
            b = tile_i % 2
            ohb_ = ohb2[b]
            bc = nb()
            op("pe", mmgroup([(ps[bc][:, 0:32], tri, ohb_, True, True), (ps[bc][:, 32:64], onesb, ohb_, True, True)]),
               r=[("ohb", b), "cB"], w=[("ps", bc)])
            tt("dve", posn, ps[bc][:, 0:32], base, ALU.add, r=[("ps", bc), "base"], w=["posn"])
            for k_ in range(2):
                tt("dve", sm4, oh_all[:, tile_i, k_, :], posn, ALU.mult, r=[("oh", tile_i), "posn"], w=["sm4"])
                op("dve", lambda e, k_=k_: e.tensor_reduce(out=rk_all[:, tile_i, k_:k_ + 1], in_=sm4, axis=AX.X, op=ALU.add),
                   r=["sm4"], w=["rk"])
            tt("dve", base, base, ps[bc][:, 32:64], ALU.add, r=[("ps", bc), "base"], w=["base"])

        for g in range(8):
            t0 = g * 512
            for ti in range(4):
                b = ti % 2
                dma("sp", rgt[b], rg_d[t0 + ti * 128:t0 + (ti + 1) * 128, :], w=[("rgt", b)])
                bk = nb()
                op("pe", trgroup([(psb[bk][:, kc * 128:(kc + 1) * 128], rgt[b][:, kc * 128:(kc + 1) * 128]) for kc in range(8)], identb),
                   r=[("rgt", b), "cB"], w=[("ps", bk)])
                cp("act", rgT[:, :, ti * 128:(ti + 1) * 128], psb[bk][:, :].rearrange("p (k t) -> p k t", k=8), r=[("ps", bk)], w=["rgT"])
            dma("sp", hyT, hy_d[:, t0:t0 + 512].rearrange("(k p) t -> p k t", p=128), w=["hyT"])
            for fo in range(8):
                b = fo % 2
                dma("sp", gt_[b], gates_d[fo * 128:(fo + 1) * 128, t0:t0 + 512], w=[("gt", b)])
                dma("sp", gh_[b], gates_d[1024 + fo * 128:1024 + (fo + 1) * 128, t0:t0 + 512], w=[("gh", b)])
                ba = nb()
                op("pe", mmgroup([(ps[ba][:, :], rwo[:, kc, fo * 128:(fo + 1) * 128], rgT[:, kc, :], kc == 0, kc == 7) for kc in range(8)]),
                   r=["rwo", "rgT"], w=[("ps", ba)])
                bb_ = nb()
                op("pe", mmgroup([(ps[bb_][:, :], hwo[:, kc, fo * 128:(fo + 1) * 128], hyT[:, kc, :], kc == 0, kc == 3) for kc in range(4)]),
                   r=["hwo", "hyT"], w=[("ps", bb_)])
                tt("dve", m1, ps[ba][:, :], gt_[b], ALU.mult, r=[("ps", ba), ("gt", b)], w=["m1"])
                tt("dve", m2, ps[bb_][:, :], gh_[b], ALU.mult, r=[("ps", bb_), ("gh", b)], w=["m2"])
                tt("dve", mixT[:, fo, :], m1, m2, ALU.add, r=["m1", "m2"], w=["mixT"])
            for ti in range(4):
                tile_i = g * 4 + ti
                T1(tile_i, ti)
                if tile_i >= 1:
                    T2(tile_i - 1)
                if tile_i >= 2:
                    T3(tile_i - 2)
                if tile_i >= 3:
                    T4(tile_i - 3)
        T2(31)
        T3(30)
        T4(29)
        T3(31)
        T4(30)
        T4(31)

        S.barrier()
        st["off"] = mark
        cRt = alloc(host_consts()["cR"].shape[1], F32)
        dma("sp", cRt, cR_d, w=["cR"])

        def cr(name, shp):
            o, n = CR[name]
            v = cRt[:, o:o + n]
            if shp == 3:
                return v.rearrange("p (a b) -> p a b", b=(16 if name == "thr" else 32))
            return v
        big = alloc(NSLOT * 32, F32)
        nblk = alloc(32, F32)
        pend = alloc(32, F32)
        pstart = alloc(32, F32)
        esl = alloc(NSLOT, F32)
        idxf = alloc(NSLOT * 5, F32).rearrange("p (s k) -> p s k", k=5)
        b3 = big[:, 0:512].rearrange("p (e j) -> p e j", j=16)
        tt("dve", b3, base.unsqueeze(2).to_broadcast([128, 32, 16]), cr("thr", 3), ALU.is_gt, r=["base", "cR"], w=["big"])
        op("dve", lambda e: e.tensor_reduce(out=nblk, in_=b3, axis=AX.X, op=ALU.add), r=["big"], w=["nblk"])
        ts("dve", nblk, nblk, float(SLOTR), None, ALU.mult, None, r=["nblk"], w=["nblk"])
        b4 = big[:, 0:1024].rearrange("p (e f) -> p e f", f=32)
        tt("dve", b4, nblk.unsqueeze(1).to_broadcast([128, 32, 32]), cr("le", 3), ALU.mult, r=["nblk", "cR"], w=["big"])
        op("dve", lambda e: e.tensor_reduce(out=pend, in_=b4, axis=AX.X, op=ALU.add), r=["big"], w=["pend"])
        tt("dve", pstart, pend, nblk, ALU.subtract, r=["pend", "nblk"], w=["pstart"])
        b5 = big.rearrange("p (s e) -> p s e", e=32)
        tt("dve", b5, pend.unsqueeze(1).to_broadcast([128, NSLOT, 32]), cr("slotv", 3), ALU.is_le, r=["pend", "cR"], w=["big"])
        op("dve", lambda e: e.tensor_reduce(out=esl, in_=b5, axis=AX.X, op=ALU.add), r=["big"], w=["esl"])
        ts("dve", esl, esl, 31.0, None, ALU.min, None, r=["esl"], w=["esl"])
        pidx = ca("pidx")
        ts("dve", idxf[:, :, 0], esl, 128.0, pidx, ALU.mult, ALU.add, r=["esl", "cA"], w=["idxf"])
        for hc in range(4):
            ts("dve", idxf[:, :, 1 + hc], esl, 512.0, pidx, ALU.mult, ALU.add, r=["esl", "cA"], w=["idxf"])
            ts("dve", idxf[:, :, 1 + hc], idxf[:, :, 1 + hc], float(hc * 128), None, ALU.add, None, r=["idxf"], w=["idxf"])
        cp("dve", idxw.rearrange("p (s k) -> p s k", k=5), idxf, r=["idxf"], w=["idxw"])
        dtmp = alloc(NT * 2, F32).rearrange("p (t k) -> p t k", t=NT)
        bigv = big.rearrange("p (s e) -> p s e", e=32)
        tt("dve", bigv, oh_all.rearrange("p t k e -> p (t k) e"), pstart.unsqueeze(1).to_broadcast([128, NT * 2, 32]), ALU.mult,
           r=["pstart"], w=["big"])
        op("dve", lambda e: e.tensor_reduce(out=dtmp.rearrange("p t k -> p (t k)"), in_=bigv, axis=AX.X, op=ALU.add), r=["big"], w=["dtmp"])
        tt("dve", dtmp, dtmp, rk_all, ALU.add, r=["dtmp", "rk"], w=["dtmp"])
        cp("dve", destv, dtmp, r=["dtmp"], w=["dest"])
        for tile_i in range(NT):
            for k_ in range(2):
                S.add("pool", lambda e, tile_i=tile_i, k_=k_: e.indirect_dma_start(
                    out=xb_d, out_offset=bass.IndirectOffsetOnAxis(ap=destv[:, tile_i, k_:k_ + 1], axis=0),
                    in_=h2all[:, tile_i, :], in_offset=None), r=["dest", ("h2all", tile_i)], w=["xb"], dma=True)

        assert bgpos[0] == 96, bgpos
        S.barrier(include_bg=True)
        st["off"] = st["pers"]
        xblk = [alloc(D, BF16) for _ in range(4)]
        xT = [alloc(8 * 256, BF16).rearrange("p (k r) -> p k r", k=8) for _ in range(2)]
        W1 = [alloc(8 * 512, BF16).rearrange("p (k n) -> p k n", k=8) for _ in range(3)]
        W3 = [alloc(8 * 512, BF16).rearrange("p (k n) -> p k n", k=8) for _ in range(3)]
        W2 = [alloc(4 * D, BF16).rearrange("p (k n) -> p k n", k=4) for _ in range(3)]
        sg1 = [alloc(512, F32), alloc(512, F32)]
        a1 = [alloc(512, F32), alloc(512, F32)]
        actT = [alloc(4 * 256, BF16).rearrange("p (k r) -> p k r", k=4) for _ in range(2)]
        yst = [alloc(D, F32) for _ in range(4)]
        idxv = idxw.rearrange("p (s k) -> p s k", k=5)

        def stageA(s):
            wb = s % 3
            xb_ = s % 2
            S.add("pool", lambda e: e.indirect_dma_start(out=W1[wb].rearrange("p k n -> p (k n)"), out_offset=None, in_=ew1b_d,
                  in_offset=bass.IndirectOffsetOnAxis(ap=idxv[:, s, 0:1], axis=0)), r=["idxw"], w=[("W1", wb)], dma=True)
            S.add("pool", lambda e: e.indirect_dma_start(out=W3[wb].rearrange("p k n -> p (k n)"), out_offset=None, in_=ew3b_d,
                  in_offset=bass.IndirectOffsetOnAxis(ap=idxv[:, s, 0:1], axis=0)), r=["idxw"], w=[("W3", wb)], dma=True)
            for hc in range(4):
                S.add("pool", lambda e, hc=hc: e.indirect_dma_start(out=W2[wb][:, hc, :], out_offset=None, in_=ew2b_d,
                      in_offset=bass.IndirectOffsetOnAxis(ap=idxv[:, s, 1 + hc:2 + hc], axis=0)), r=["idxw"], w=[("W2", wb)], dma=True)
            for rt in range(2):
                r0 = s * SLOTR + rt * 128
                xi = xb_ * 2 + rt
                dma("sp", xblk[xi], xb_d[r0:r0 + 128, :], r=["xb"], w=[("xblk", xi)])
                bk = nb()
                xv = xblk[xi].rearrange("p (f k) -> p k f", k=8)
                op("pe", trgroup([(psb[bk][:, kk * 128:(kk + 1) * 128], xv[:, kk, :]) for kk in range(8)], identb),
                   r=[("xblk", xi), "cB"], w=[("ps", bk)])
                cp("act", xT[xb_][:, :, rt * 128:(rt + 1) * 128], psb[bk][:, :].rearrange("p (k r) -> p k r", k=8), r=[("ps", bk)], w=[("xT", xb_)])

        def stageB(s):
            wb = s % 3
            xb_ = s % 2
            for hp in range(2):
                b1_, b3_ = nb(), nb()
                l1_, l3_ = [], []
                for hh in range(2):
                    hc = hp * 2 + hh
                    for kk in range(8):
                        l1_.append((ps[b1_][:, hh * 256:(hh + 1) * 256], W1[wb][:, kk, hc * 128:(hc + 1) * 128], xT[xb_][:, kk, :], kk == 0, kk == 7))
                        l3_.append((ps[b3_][:, hh * 256:(hh + 1) * 256], W3[wb][:, kk, hc * 128:(hc + 1) * 128], xT[xb_][:, kk, :], kk == 0, kk == 7))
                op("pe", mmgroup(l1_), r=[("W1", wb), ("xT", xb_)], w=[("ps", b1_)])
                op("pe", mmgroup(l3_), r=[("W3", wb), ("xT", xb_)], w=[("ps", b3_)])
                act(sg1[hp], ps[b1_][:, :], AF.Sigmoid, r=[("ps", b1_)], w=[("sg1", hp)])
                tt("dve", a1[hp], ps[b1_][:, :], sg1[hp], ALU.mult, r=[("ps", b1_), ("sg1", hp)], w=[("a1", hp)])
                tt("dve", actT[xb_][:, hp * 2:hp * 2 + 2, :], a1[hp].rearrange("p (k r) -> p k r", k=2), ps[b3_][:, :].rearrange("p (k r) -> p k r", k=2),
                   ALU.mult, r=[("a1", hp), ("ps", b3_)], w=[("actT", xb_)])

        def stageC(s):
            wb = s % 3
            xb_ = s % 2
            for rt in range(2):
                yi = xb_ * 2 + rt
                for nh in range(2):
                    bk = nb()
                    op("pe", mmgroup([(ps[bk][:, :], actT[xb_][:, hc, rt * 128:(rt + 1) * 128], W2[wb][:, hc, nh * 512:(nh + 1) * 512], hc == 0, hc == 3)
                                      for hc in range(4)]), r=[("actT", xb_), ("W2", wb)], w=[("ps", bk)])
                    cp("act" if nh == 0 else "dve", yst[yi][:, nh * 512:(nh + 1) * 512], ps[bk][:, :], r=[("ps", bk)], w=[("yst", yi)])
                r0 = s * SLOTR + rt * 128
                dma("sp", yb_d[r0:r0 + 128, :], yst[yi], r=[("yst", yi)], w=["yb"])
        stageA(0)
        for s in range(NSLOT):
            if s + 1 < NSLOT:
                stageA(s + 1)
            stageB(s)
            if s >= 1:
                stageC(s - 1)
        stageC(NSLOT - 1)

        phase_reset()
        r1 = [alloc(D, F32) for _ in range(4)]
        r2 = [alloc(D, F32) for _ in range(4)]
        x2r = [alloc(D, F32) for _ in range(4)]
        yo = [alloc(D, F32) for _ in range(4)]
        ss3 = alloc(4, F32)
        junk3 = alloc(D, BF16)
        GT2 = alloc(D, F32)
        GF = alloc(D, F32)
        dma("sp", GT2, mod_d[:, 5120:6144], w=["modL"])
        dma("sp", GF, gains_d[:, 2048:3072], w=["gains"])
        outs = []
        for tile_i in range(NT):
            b = tile_i % 4
            r0 = tile_i * 128
            S.add("pool", lambda e, tile_i=tile_i, b=b: e.indirect_dma_start(out=r1[b], out_offset=None, in_=yb_d,
                  in_offset=bass.IndirectOffsetOnAxis(ap=destv[:, tile_i, 0:1], axis=0)), r=["yb", "dest"], w=[("r1", b)], dma=True)
            S.add("pool", lambda e, tile_i=tile_i, b=b: e.indirect_dma_start(out=r2[b], out_offset=None, in_=yb_d,
                  in_offset=bass.IndirectOffsetOnAxis(ap=destv[:, tile_i, 1:2], axis=0)), r=["yb", "dest"], w=[("r2", b)], dma=True)
            dma("sp", x2r[b], x2_d[r0:r0 + 128, :], w=[("x2r", b)])
            ts("dve", yo[b], r1[b], wtsv[:, tile_i, 0:1], None, ALU.mult, None, r=[("r1", b), "wts"], w=[("yo", b)])
            stt("dve", yo[b], r2[b], wtsv[:, tile_i, 1:2], yo[b], ALU.mult, ALU.add, r=[("r2", b), ("yo", b), "wts"], w=[("yo", b)])
            tt("dve", yo[b], yo[b], GT2, ALU.mult, r=[("yo", b), "modL"], w=[("yo", b)])
            tt("dve", yo[b], yo[b], x2r[b], ALU.add, r=[("yo", b), ("x2r", b)], w=[("yo", b)])
            sscol = ss3[:, b:b + 1]
            act(junk3, yo[b], AF.Square, r=[("yo", b)], w=["junk3", ("ss3", b)], accum=sscol)
            rstd_from_ss(sscol, D, ("ss3", b))
            stt("dve", yo[b], yo[b], sscol, GF, ALU.mult, ALU.mult, r=[("yo", b), ("ss3", b), "gains"], w=[("yo", b)])
            outs.append(dma("sp", out_d[r0:r0 + 128, :], yo[b], r=[("yo", b)]))
        S.emit(final_wait_ops=outs)
    return nc


def _bc(v, n=128):
    return np.ascontiguousarray(np.broadcast_to(np.asarray(v, np.float32).reshape(1, -1), (n, np.asarray(v).size)))


def _fm(v):
    v = np.asarray(v, np.float32).reshape(-1)
    return np.ascontiguousarray(v.reshape(-1, 128).T)


def make_in_maps(inp, cores):
    hc = host_consts()
    g = {k: np.asarray(v) for k, v in inp.items()}
    shared = dict(
        ada_w=np.ascontiguousarray(g["ada_w"][0]),
        ada_b_b=_bc(g["ada_b"][0]),
        gains_b=np.concatenate([_bc(g["norm1_g"][0]), _bc(g["norm2_g"][0]), _bc(g["final_norm_g"])], 1),
        w_in=np.ascontiguousarray(g["w_in"][0]),
        b_in_fm=_fm(g["b_in"][0]),
        b_in_b=_bc(g["b_in"][0][512:3072]),
        dlog_b=_bc(g["ret_decay_logit"][0].reshape(-1)),
        ret_w_o=np.ascontiguousarray(g["ret_w_o"][0]),
        hy_w_o=np.ascontiguousarray(g["hy_w_o"][0]),
        w_out=np.ascontiguousarray(g["w_out"][0]),
        hyskip=_fm(g["hy_skip"][0]),
        hw1=np.ascontiguousarray(g["hy_ffn_w1"][0]),
        hvec=np.ascontiguousarray(np.stack([g["hy_ffn_b1"][0], g["hy_ffn_freq"][0], g["hy_ffn_b2"][0], np.zeros(64, np.float32)], 1)),
        hw2=np.ascontiguousarray(g["hy_ffn_w2"][0]),
        hw3=np.ascontiguousarray(g["hy_ffn_w3"][0]),
        wr=np.ascontiguousarray(np.concatenate([g["router_group_w"][0], g["router_expert_w"][0]], 1)),
        br_b=_bc(np.concatenate([g["router_group_b"][0], g["router_expert_b"][0]])),
        ew1=np.ascontiguousarray(g["expert_w1"][0].reshape(32 * 128, 8 * 512)),
        ew3=np.ascontiguousarray(g["expert_w3"][0].reshape(32 * 128, 8 * 512)),
        ew2=np.ascontiguousarray(g["expert_w2"][0].reshape(32 * 512, D)),
        cA=hc["cA"], cR=hc["cR"], cB=hc["cB"], ftab=hc["ftab"], itab=hc["itab"], rope=hc["rope"],
        featsT=hc["featsT"], tvec=hc["tvec"],
    )
    cw = g["hy_conv_w"][0]
    cbias = g["hy_conv_b"][0]
    hyc = np.zeros((128, 12, 5), np.float32)
    for j in range(3):
        hyc[:, :, j] = cw[j].reshape(12, 128).T
    hyc[:, :, 3] = cbias.reshape(12, 128).T
    shared["hyc"] = hyc.reshape(128, 60)
    maps = []
    for b in cores:
        m = dict(shared)
        m["x"] = np.ascontiguousarray(g["x"][b])
        m["ctx"] = np.ascontiguousarray(g["ctx"][b])
        cv = np.zeros((128, 8, 2), np.float32)
        cv[:, :, 0] = g["c"][b].reshape(8, 128).T
        cv[:, :, 1] = g["c_ctx"].reshape(8, 128).T
        m["cvec"] = cv.reshape(128, 16)
        maps.append(m)
    return maps


def kernel(**inputs):
    nc = build_program(debug=False)
    maps = make_in_maps(inputs, list(range(8)))
    res = run_bass_kernel_spmd(nc, maps, core_ids=list(range(8)))
    return np.stack([np.asarray(r["out"], np.float32) for r in res.results], 0)
```

```python
import contextlib
import math
import numpy as np
import ml_dtypes
import concourse.bass as bass
import concourse.mybir as mybir
from concourse.bass_utils import run_bass_kernel_spmd

F32 = mybir.dt.float32
BF16 = mybir.dt.bfloat16
I32 = mybir.dt.int32
U8 = mybir.dt.uint8
AF = mybir.ActivationFunctionType
ALU = mybir.AluOpType
AX = mybir.AxisListType
BF = ml_dtypes.bfloat16

L = 4096
D = 1024
NT = 32
EPS = 1e-6
INW = 6656
NSLOT = 64
SLOTR = 256
NROWS = NSLOT * SLOTR

ENGS = ("pe", "act", "dve", "pool", "sp")
NDMASEM = 8
SAME_ENGINE_WAR_SYNC = True
QSIZE = {"bg": 16, "sp": 16}


class Sched:
    def __init__(self, nc, same_engine_sync=True):
        self.nc = nc
        self.ops = []
        self.lastw = {}
        self.readers = {}
        self.same_engine_sync = same_engine_sync
        self.bar_deps = set()
        self.bar_pending = {}

    def add(self, eng, fn, r=(), w=(), dma=False, q=None, bg=False):
        deps = set()
        war = set()
        for t in r:
            if t in self.lastw:
                deps.add(self.lastw[t])
        for t in w:
            if t in self.lastw:
                deps.add(self.lastw[t])
            for x in self.readers.get(t, ()):
                if x not in deps:
                    war.add(x)
        war -= deps
        deps |= war
        idx = len(self.ops)
        if self.bar_pending.get(eng, False):
            deps |= self.bar_deps
            self.bar_pending[eng] = False
        deps.discard(idx)
        self.ops.append(dict(eng=eng, fn=fn, deps=deps, war=war, dma=dma, sig=False, q=(q or eng), bg=bg))
        for t in r:
            self.readers.setdefault(t, []).append(idx)
        for t in w:
            self.lastw[t] = idx
            self.readers[t] = []
        return idx

    def barrier(self, include_bg=False):
        last = {}
        dmas = {}
        for i, o in enumerate(self.ops):
            if o["dma"]:
                if o["bg"] and not include_bg:
                    continue
                dmas.setdefault(o["q"], []).append(i)
            else:
                last[o["eng"]] = i
        bd = set(last.values())
        for e, l in dmas.items():
            bd |= set(l[-QSIZE.get(e, NDMASEM):])
        self.bar_deps = bd
        self.bar_pending = {e: True for e in ENGS}
        self.lastw = {}
        self.readers = {}

    def emit(self, final_wait_ops=()):
        nc = self.nc
        ops = self.ops
        for i, o in enumerate(ops):
            nd = set()
            for d in o["deps"]:
                od = ops[d]
                if od["eng"] == o["eng"] and not od["dma"]:
                    if o["eng"] in ("pe", "sp"):
                        continue
                    if not self.same_engine_sync:
                        continue
                    if d in o["war"] and not SAME_ENGINE_WAR_SYNC:
                        continue
                nd.add(d)
            o["deps"] = nd
            for d in nd:
                ops[d]["sig"] = True
        for d in final_wait_ops:
            ops[d]["sig"] = True
        cnt = {e: 0 for e in ENGS}
        dcnt = {}
        for o in ops:
            if o["dma"]:
                o["dn"] = dcnt.get(o["q"], 0)
                dcnt[o["q"]] = o["dn"] + 1
            elif o["sig"]:
                cnt[o["eng"]] += 1
                o["sn"] = cnt[o["eng"]]
        with contextlib.ExitStack() as es:
            csem = {e: es.enter_context(nc.semaphore("c_" + e)) for e in ENGS if e != "sp"}
            dsem = {e: [es.enter_context(nc.semaphore("d_%s%d" % (e, i))) for i in range(QSIZE.get(e, NDMASEM))]
                    for e in dcnt}
            block = es.enter_context(nc.Block())

            def target(d):
                od = ops[d]
                if od["dma"]:
                    n = od["dn"]
                    K_ = QSIZE.get(od["q"], NDMASEM)
                    return dsem[od["q"]][n % K_], 16 * (n // K_ + 1)
                return csem[od["eng"]], od["sn"]

            def run(ename, eobj):
                seen = {}
                for i, o in enumerate(ops):
                    if o["eng"] != ename:
                        continue
                    waits = {}
                    for d in o["deps"]:
                        s, v = target(d)
                        key = id(s)
                        if seen.get(key, 0) >= v:
                            continue
                        if key not in waits or waits[key][1] < v:
                            waits[key] = (s, v)
                    K_ = QSIZE.get(o["q"], NDMASEM)
                    if o["dma"] and o["dn"] >= K_:
                        s = dsem[o["q"]][o["dn"] % K_]
                        v = 16 * (o["dn"] // K_)
                        key = id(s)
                        if seen.get(key, 0) < v and (key not in waits or waits[key][1] < v):
                            waits[key] = (s, v)
                    for key, (s, v) in waits.items():
                        eobj.wait_ge(s, v)
                        seen[key] = v
                    ins = o["fn"](eobj)
                    if o["dma"]:
                        s, v = target(i)
                        ins.then_inc(s, 16)
                    elif o["sig"]:
                        ins.then_inc(csem[ename], 1)
                if ename == "sp":
                    for d in final_wait_ops:
                        s, v = target(d)
                        eobj.wait_ge(s, v)

            @block.sync
            def _(e):
                run("sp", e)

            @block.scalar
            def _(e):
                run("act", e)

            @block.vector
            def _(e):
                run("dve", e)

            @block.gpsimd
            def _(e):
                run("pool", e)

            @block.tensor
            def _(e):
                run("pe", e)


def _fft_tables():
    j = np.arange(128)[:, None]
    k1 = np.arange(128)[None, :]
    phi = 2 * np.pi * (k1 + 0.5) * j / 256.0
    E1c = np.cos(phi)
    E1s = -np.sin(phi)
    a = np.arange(32)[:, None]
    k2 = np.arange(32)[None, :]
    FT = np.zeros((3, 32, 128, 128))
    for q in range(32):
        for m in range(4):
            kk = 4 * q + m
            th = -2 * np.pi * ((kk + 0.5) * a / 8192.0 + k2 * a / 32.0)
            sl = slice(32 * m, 32 * m + 32)
            FT[0, q, sl, sl] = np.cos(th)
            FT[1, q, sl, sl] = np.sin(th)
            FT[2, q, sl, sl] = -np.sin(th)
    IT = np.transpose(FT, (0, 1, 3, 2)).copy()
    return E1c, E1s, FT, IT


CA = {}
CR = {}
CB = {}


def _pack(specs, table):
    off = 0
    cols = []
    for name, arr in specs:
        arr = np.asarray(arr, np.float64).reshape(128, -1)
        table[name] = (off, arr.shape[1])
        off += arr.shape[1]
        cols.append(arr)
    return np.concatenate(cols, axis=1)


_CONST_CACHE = {}


def host_consts():
    if _CONST_CACHE:
        return _CONST_CACHE
    p = np.arange(128)
    ident = np.eye(128)
    partner = np.where((p % 64) < 32, p + 32, p - 32)
    perm = np.zeros((128, 128))
    perm[partner, p] = 1.0
    jj = p[:, None]
    ii = p[None, :]
    diff_f = np.maximum(ii - jj, 0)
    mask_f = (ii >= jj) * 1.0
    diff_b = np.maximum(jj - ii, 0)
    mask_b = (jj > ii) * 1.0
    pcol = np.stack([127 - p, p], 1)
    prow = np.concatenate([np.tile(p + 1, (128, 1)), np.tile(128 - p, (128, 1))], 1)
    slow = abs(math.log(1e-2)) / 1.5
    fast = abs(math.log(1e-2)) / 0.3
    deltas = np.tile(np.linspace(slow, fast, 512, dtype=np.float32).astype(np.float64), 2)
    negdelta = -deltas.reshape(8, 128).T
    eidx = np.tile(np.arange(32), (128, 1))
    cA = _pack([("identf", ident), ("permf", perm), ("diff_f", diff_f), ("mask_f", mask_f), ("diff_b", diff_b),
                ("mask_b", mask_b), ("pcol", pcol), ("prow", prow), ("negdelta", negdelta), ("eidx", eidx),
                ("pidx", p[:, None]), ("onesf", np.ones((128, 128)))], CA).astype(np.float32)
    thr = np.tile((SLOTR * np.arange(16))[None, None, :], (128, 32, 1))
    le = np.tile((np.arange(32)[None, :] <= np.arange(32)[:, None])[None] * 1.0, (128, 1, 1))
    lt = np.tile((np.arange(32)[None, :] < np.arange(32)[:, None])[None] * 1.0, (128, 1, 1))
    slotv = np.tile((SLOTR * np.arange(NSLOT))[None, :, None], (128, 1, 32))
    cR = _pack([("thr", thr), ("le", le), ("lt", lt), ("slotv", slotv)], CR).astype(np.float32)
    E1c, E1s, FT, IT = _fft_tables()
    tri = (jj < ii) * 1.0
    cB = _pack([("identb", ident), ("tri", tri), ("onesb", np.ones((128, 128))), ("E1c", E1c), ("E1s", E1s),
                ("E1cT", E1c.T), ("E1sT", E1s.T)], CB).astype(BF)
    ftab = np.transpose(FT, (2, 0, 1, 3)).reshape(128, 3 * 32 * 128).astype(BF)
    itab = np.transpose(IT, (2, 0, 1, 3)).reshape(128, 3 * 32 * 128).astype(BF)
    rows = L // 64
    r, col = np.meshgrid(np.arange(rows, dtype=np.float32), np.arange(64, dtype=np.float32), indexing="ij")
    inv_freq = (np.float32(10000.0) ** (-np.arange(32, dtype=np.float32) / np.float32(32))).astype(np.float32)
    ang_row = (r.reshape(-1)[:, None] * inv_freq).astype(np.float32)
    ang_col = (col.reshape(-1)[:, None] * inv_freq).astype(np.float32)
    cos_t = np.concatenate([np.cos(ang_row), np.cos(ang_row), np.cos(ang_col), np.cos(ang_col)], 1).T
    sin_t = np.concatenate([-np.sin(ang_row), np.sin(ang_row), -np.sin(ang_col), np.sin(ang_col)], 1).T
    rope = np.concatenate([cos_t, sin_t], 1).astype(np.float32)
    t = (np.arange(L, dtype=np.float32) / np.float32(L)).astype(np.float32)
    bands = np.linspace(1e-4, 15, 16, dtype=np.float32)
    phase = (2.0 * math.pi * t[:, None] * bands[None, :]).astype(np.float32)
    feats = np.concatenate([t[:, None], np.cos(phase), -np.sin(phase)], -1).astype(np.float32)
    featsT = np.ascontiguousarray(feats.T)
    tvec = np.tile(t[None, :], (128, 1)).astype(np.float32)
    _CONST_CACHE.update(cA=cA, cR=cR, cB=cB, ftab=ftab, itab=itab, rope=rope, featsT=featsT, tvec=tvec)
    return _CONST_CACHE


def build_program(debug=False):
    host_consts()
    nc = bass.Bass("TRN2", target_bir_lowering=False)
    dk = "ExternalOutput" if debug else "Internal"

    def din(name, shape, dt=F32):
        return nc.dram_tensor(name, list(shape), dt, kind="ExternalInput").ap()

    def dscr(name, shape, dt=F32, dbg=False):
        return nc.dram_tensor(name, list(shape), dt, kind=(dk if dbg else "Internal")).ap()

    x_d = din("x", [L, D])
    ctx_d = din("ctx", [256, D])
    cvec_d = din("cvec", [128, 16])
    ada_w_d = din("ada_w", [D, 6144])
    ada_b_d = din("ada_b_b", [128, 6144])
    gains_d = din("gains_b", [128, 3 * D])
    w_in_d = din("w_in", [D, INW])
    b_in_fm_d = din("b_in_fm", [128, 52])
    b_in_b_d = din("b_in_b", [128, 2560])
    dlog_d = din("dlog_b", [128, 8])
    ret_w_o_d = din("ret_w_o", [D, D])
    hy_w_o_d = din("hy_w_o", [512, D])
    w_out_d = din("w_out", [D, D])
    hyc_d = din("hyc", [128, 12 * 5])
    hyskip_d = din("hyskip", [128, 4])
    hw1_d = din("hw1", [33, 64])
    hvec_d = din("hvec", [64, 4])
    hw2_d = din("hw2", [64, 64])
    hw3_d = din("hw3", [64, 1024])
    wr_d = din("wr", [D, 36])
    br_d = din("br_b", [128, 36])
    ew1_d = din("ew1", [32 * 128, 8 * 512])
    ew3_d = din("ew3", [32 * 128, 8 * 512])
    ew2_d = din("ew2", [32 * 512, D])
    cA_d = din("cA", [128, host_consts()["cA"].shape[1]])
    cR_d = din("cR", [128, host_consts()["cR"].shape[1]])
    cB_d = din("cB", [128, host_consts()["cB"].shape[1]], BF16)
    ftab_d = din("ftab", [128, 3 * 32 * 128], BF16)
    itab_d = din("itab", [128, 3 * 32 * 128], BF16)
    rope_d = din("rope", [128, 2 * L])
    featsT_d = din("featsT", [33, L])
    tvec_d = din("tvec", [128, L])
    out_d = nc.dram_tensor("out", [L, D], F32, kind="ExternalOutput").ap()

    qT_d = dscr("qT_s", [4, 128, L], BF16, True)
    kT_d = dscr("kT_s", [4, 128, L], BF16, True)
    kt_d = dscr("kt_s", [L, 512], BF16)
    v_d = dscr("v_s", [L, D], BF16, True)
    gs_d = dscr("gs_s", [L, D], BF16, True)
    u_d = dscr("u_s", [1536, L], BF16, True)
    gates_d = dscr("gates_s", [2048, L], BF16, True)
    rg_d = dscr("rg_s", [L, D], BF16, True)
    filt_d = dscr("filt_s", [1024, L], F32, True)
    cd_d = dscr("cd_s", [2, 128 * 32 * 512], BF16)
    hd_d = dscr("hd_s", [2, 128, 32 * 512], F32)
    hy_d = dscr("hy_s", [512, L], BF16, True)
    x2_d = dscr("x2_s", [L, D], F32, True)
    xb_d = dscr("xb_s", [NROWS, D], BF16)
    yb_d = dscr("yb_s", [NROWS, D], F32)
    dbg_d = dscr("dbg_s", [128, 4096], F32, True)
    mod_d = dscr("mod_s", [128, 8192], F32)
    z_d = dscr("z_s", [512, L], BF16)
    hs_d = dscr("hs_s", [2, 512, L], BF16)
    ew1b_d = dscr("ew1b_s", [32 * 128, 8 * 512], BF16)
    ew3b_d = dscr("ew3b_s", [32 * 128, 8 * 512], BF16)
    ew2b_d = dscr("ew2b_s", [32 * 512, D], BF16)

    ARENA = 204 * 1024
    with contextlib.ExitStack() as es:
        arena = es.enter_context(nc.sbuf_tensor("arena", [128, ARENA], U8))
        ps = [es.enter_context(nc.psum_tensor("ps%d" % i, [128, 512], F32)) for i in range(8)]
        psb = [p_[:, :].bitcast(BF16) for p_ in ps]
        S = Sched(nc)
        st = dict(off=0, pers=0, bank=0)

        def alloc(n, dt, pers=False):
            sz = 2 if dt == BF16 else 4
            nb_ = (n * sz + 63) // 64 * 64
            a = arena[:, st["off"]:st["off"] + n * sz].bitcast(dt)
            st["off"] += nb_
            assert st["off"] <= ARENA, ("SBUF arena overflow", st["off"])
            if pers:
                st["pers"] = st["off"]
            return a

        def phase_reset():
            S.barrier()
            st["off"] = st["pers"]

        def nb():
            st["bank"] = (st["bank"] + 1) % 8
            return st["bank"]

        def dma(eng, out, in_, r=(), w=()):
            return S.add(eng, lambda e: e.dma_start(out=out, in_=in_), r=r, w=w, dma=True)

        def op(eng, f, r=(), w=()):
            return S.add(eng, f, r=r, w=w)

        def mm(out, lhsT, rhs, start, stop):
            return lambda e: e.matmul(out, lhsT=lhsT, rhs=rhs, start=start, stop=stop)

        def mmgroup(lst):
            def f(e):
                ins = None
                for (o, l, r_, s0, s1) in lst:
                    ins = e.matmul(o, lhsT=l, rhs=r_, start=s0, stop=s1)
                return ins
            return f

        def trgroup(lst, ident):
            def f(e):
                ins = None
                for (o, i_) in lst:
                    ins = e.transpose(out=o, in_=i_, identity=ident)
                return ins
            return f

        def act(out, in_, func, r, w, bias=None, scale=None, accum=None, eng="act"):
            kw = {}
            if bias is not None:
                kw["bias"] = bias
            if scale is not None:
                kw["scale"] = scale
            if accum is not None:
                kw["accum_out"] = accum
            return S.add(eng, lambda e: e.activation(out=out, in_=in_, func=func, **kw), r=r, w=w)

        def tt(eng, out, in0, in1, o, r, w):
            return S.add(eng, lambda e: e.tensor_tensor(out=out, in0=in0, in1=in1, op=o), r=r, w=w)

        def ts(eng, out, in0, s1, s2, o0, o1, r, w):
            if s2 is None:
                return S.add(eng, lambda e: e.tensor_scalar(out=out, in0=in0, scalar1=s1, scalar2=None, op0=o0), r=r, w=w)
            return S.add(eng, lambda e: e.tensor_scalar(out=out, in0=in0, scalar1=s1, scalar2=s2, op0=o0, op1=o1), r=r, w=w)

        def stt(eng, out, in0, sc, in1, o0, o1, r, w):
            return S.add(eng, lambda e: e.scalar_tensor_tensor(out=out, in0=in0, scalar=sc, in1=in1, op0=o0, op1=o1), r=r, w=w)

        def cp(eng, out, in_, r, w):
            if eng == "act":
                return S.add(eng, lambda e: e.copy(out=out, in_=in_), r=r, w=w)
            return S.add(eng, lambda e: e.tensor_copy(out=out, in_=in_), r=r, w=w)

        def rstd_from_ss(ss, n, tag):
            act(ss, ss, AF.Sqrt, r=[tag], w=[tag], scale=1.0 / n, bias=epsc)
            op("dve", lambda e: e.reciprocal(out=ss, in_=ss), r=[tag], w=[tag])

        cA = alloc(host_consts()["cA"].shape[1], F32, pers=True)
        cBt = alloc(host_consts()["cB"].shape[1], BF16, pers=True)
        epsc = alloc(1, F32, pers=True)
        dec = alloc(8 + 8 + 8, F32, pers=True)
        s0 = alloc(8 * 256, F32, pers=True)
        dest = alloc(64, I32, pers=True)
        wts = alloc(64, F32, pers=True)
        idxw = alloc(NSLOT * 5, I32, pers=True)
        Dm = alloc(4 * 128, F32, pers=True).rearrange("p (h i) -> p h i", h=4)
        DQf = alloc(8 * 128, F32, pers=True).rearrange("p (h i) -> p h i", h=8)
        DK = alloc(8, F32, pers=True)

        def ca(name):
            o, n = CA[name]
            return cA[:, o:o + n]

        def cb(name):
            o, n = CB[name]
            return cBt[:, o:o + n]
        dma("sp", cA, cA_d, w=["cA"])
        dma("sp", cBt, cB_d, w=["cB"])
        op("dve", lambda e: e.memset(epsc, EPS), w=["epsc"])
        identb = cb("identb")
        identf = ca("identf")

        modL = alloc(6144, F32)
        gains = alloc(3 * D, F32)
        dma("sp", gains, gains_d, w=["gains"])
        cv = alloc(16, F32)
        dma("sp", cv, cvec_d, w=["cv"])
        sg = alloc(16, F32)
        act(sg, cv, AF.Sigmoid, r=["cv"], w=["sg"])
        tt("dve", cv, cv, sg, ALU.mult, r=["cv", "sg"], w=["cv"])
        rep = alloc(8 * 128 * 2, F32).rearrange("p (k j m) -> p k j m", k=8, j=2)
        for kc in range(8):
            for j_ in range(2):
                ts("dve", rep[:, kc, j_, :], ca("onesf"), cv[:, 2 * kc + j_:2 * kc + j_ + 1], None, ALU.mult, None,
                   r=["cv", "cA"], w=[("rep", kc, j_)])
        modC = alloc(2048, F32, pers=False)
        adab = alloc(6144, F32)
        dma("sp", adab, ada_b_d, w=["adab"])
        awt = [alloc(2048, F32), alloc(2048, F32)]
        ada_v = ada_w_d.rearrange("(kc p) n -> p kc n", p=128)
        for pa in range(3):
            banks = [nb() for _ in range(8 if pa == 0 else 4)]
            for kc in range(8):
                b_ = awt[kc % 2]
                dma("sp", b_, ada_v[:, kc, pa * 2048:(pa + 1) * 2048], w=[("awt", kc % 2)])
                lst = []
                for n4 in range(4):
                    lst.append((ps[banks[n4]][:, :], rep[:, kc, 0, :], b_[:, n4 * 512:(n4 + 1) * 512], kc == 0, kc == 7))
                    if pa == 0:
                        lst.append((ps[banks[4 + n4]][:, :], rep[:, kc, 1, :], b_[:, n4 * 512:(n4 + 1) * 512], kc == 0, kc == 7))
                op("pe", mmgroup(lst), r=[("awt", kc % 2)] + [("rep", kc, 0), ("rep", kc, 1)], w=[("ps", b) for b in banks])
            for n4 in range(4):
                c0 = pa * 2048 + n4 * 512
                tt("dve", modL[:, c0:c0 + 512], ps[banks[n4]][:, :], adab[:, c0:c0 + 512], ALU.add,
                   r=[("ps", banks[n4]), "adab"], w=["modL"])
                if pa == 0:
                    tt("dve", modC[:, c0:c0 + 512], ps[banks[4 + n4]][:, :], adab[:, c0:c0 + 512], ALU.add,
                       r=[("ps", banks[4 + n4]), "adab"], w=["modC"])
        A1 = modL[:, 1024:2048]
        B1 = modL[:, 0:1024]
        stt("dve", A1, A1, 1.0, gains[:, 0:1024], ALU.add, ALU.mult, r=["modL", "gains"], w=["modL"])
        A1c = modC[:, 1024:2048]
        B1c = modC[:, 0:1024]
        stt("dve", A1c, A1c, 1.0, gains[:, 0:1024], ALU.add, ALU.mult, r=["modC", "gains"], w=["modC"])
        A2 = modL[:, 4096:5120]
        B2 = modL[:, 3072:4096]
        stt("dve", A2, A2, 1.0, gains[:, 1024:2048], ALU.add, ALU.mult, r=["modL", "gains"], w=["modL"])
        dma("sp", mod_d[:, 0:6144], modL, r=["modL"], w=["mod_d"])
        dma("sp", mod_d[:, 6144:8192], modC, r=["modC"], w=["mod_d"])
        dl = alloc(8, F32)
        dma("sp", dl, dlog_d, w=["dl"])
        act(dl, dl, AF.Exp, r=["dl"], w=["dl"], scale=-1.0)
        act(dl, dl, AF.Ln, r=["dl"], w=["dl"], bias=1.0)
        LG = dec[:, 0:8]
        G128 = dec[:, 8:16]
        ts("dve", LG, dl, -1.0, None, ALU.mult, None, r=["dl"], w=["dec"])
        act(G128, LG, AF.Exp, r=["dec"], w=["dec"], scale=128.0)
        tmpd = alloc(128, F32)
        for h in range(4):
            act(tmpd, ca("diff_f"), AF.Exp, r=["cA", "dec"], w=["tmpd"], scale=LG[:, h:h + 1])
            tt("dve", Dm[:, h, :], tmpd, ca("mask_f"), ALU.mult, r=["tmpd", "cA"], w=["Dm"])
            act(tmpd, ca("diff_b"), AF.Exp, r=["cA", "dec"], w=["tmpd"], scale=LG[:, 4 + h:5 + h])
            tt("dve", tmpd, tmpd, ca("mask_b"), ALU.mult, r=["tmpd", "cA"], w=["tmpd"])
            tt("dve", Dm[:, h, :], Dm[:, h, :], tmpd, ALU.add, r=["tmpd", "Dm"], w=["Dm"])
            po, _ = CA["prow"]
            act(DQf[:, h, :], cA[:, po:po + 128], AF.Exp, r=["cA", "dec"], w=["DQ"], scale=LG[:, h:h + 1])
            act(DQf[:, 4 + h, :], cA[:, po + 128:po + 256], AF.Exp, r=["cA", "dec"], w=["DQ"], scale=LG[:, 4 + h:5 + h])
            pc, _ = CA["pcol"]
            act(DK[:, h:h + 1], cA[:, pc:pc + 1], AF.Exp, r=["cA", "dec"], w=["DK"], scale=LG[:, h:h + 1])
            act(DK[:, 4 + h:5 + h], cA[:, pc + 1:pc + 2], AF.Exp, r=["cA", "dec"], w=["DK"], scale=LG[:, 4 + h:5 + h])

        phase_reset()
        A1c = alloc(D, F32)
        B1c = alloc(D, F32)
        dma("sp", B1c, mod_d[:, 6144:7168], r=["mod_d"], w=["modC"])
        dma("sp", A1c, mod_d[:, 7168:8192], r=["mod_d"], w=["modC"])
        w_in = alloc(8 * 1536, BF16).rearrange("p (k n) -> p k n", k=8)
        w_in_v = w_in_d.rearrange("(kc p) n -> p kc n", p=128)
        for kc in range(8):
            dma("pool", w_in[:, kc, :], w_in_v[:, kc, 512:2048], w=["w_in"])
        bbb = alloc(2560, F32)
        dma("sp", bbb, b_in_b_d, w=["bbb"])
        xt = [alloc(D, F32), alloc(D, F32)]
        tmpf = alloc(D, F32)
        hb = [alloc(D, BF16), alloc(D, BF16)]
        hT = alloc(8 * 512, BF16).rearrange("p (k t) -> p k t", k=8)
        ssq = alloc(4, F32)
        junk = alloc(D, BF16)
        HT = dict(hT=hT, xt=xt, tmpf=tmpf, hb=hb, ssq=ssq, junk=junk)

        def norm_mod_T(src_ap, i, A_, B_, atag, tcol, ntile_tag):
            hT, xt, tmpf, hb, ssq, junk = HT["hT"], HT["xt"], HT["tmpf"], HT["hb"], HT["ssq"], HT["junk"]
            b = i % 2
            dma("sp", xt[b], src_ap, w=[("xt", b)])
            sscol = ssq[:, b:b + 1]
            act(junk, xt[b], AF.Square, r=[("xt", b)], w=["junk", ("ssq", b)], accum=sscol)
            rstd_from_ss(sscol, D, ("ssq", b))
            stt("dve", tmpf, xt[b], sscol, A_, ALU.mult, ALU.mult, r=[("xt", b), ("ssq", b), atag], w=["tmpf"])
            tt("dve", hb[b], tmpf, B_, ALU.add, r=["tmpf", atag], w=[("hb", b)])
            bk = nb()
            op("pe", trgroup([(psb[bk][:, kc * 128:(kc + 1) * 128], hb[b][:, kc * 128:(kc + 1) * 128]) for kc in range(8)], identb),
               r=[("hb", b), "cB"], w=[("ps", bk)])
            cp("act", hT[:, :, tcol * 128:(tcol + 1) * 128], psb[bk][:, :].rearrange("p (k t) -> p k t", k=8),
               r=[("ps", bk)], w=["hT"])

        for i in range(2):
            norm_mod_T(ctx_d[i * 128:(i + 1) * 128, :], i, A1c, B1c, "modC", i, None)
        kc_t = alloc(2 * 512, F32).rearrange("p (c n) -> p c n", c=2)
        vc_t = alloc(2 * D, BF16).rearrange("p (c n) -> p c n", c=2)
        kscale = 128.0 ** -0.5
        for i in range(2):
            for (c0, nn, which) in ((0, 512, "k"), (512, 512, "v0"), (1024, 512, "v1")):
                bk = nb()
                op("pe", mmgroup([(ps[bk][:, :], hT[:, kc, i * 128:(i + 1) * 128], w_in[:, kc, c0:c0 + 512], kc == 0, kc == 7)
                                  for kc in range(8)]), r=["hT", "w_in"], w=[("ps", bk)])
                if which == "k":
                    tt("dve", kc_t[:, i, :], ps[bk][:, :], bbb[:, 0:512], ALU.add, r=[("ps", bk), "bbb"], w=["kc_t"])
                else:
                    vo = 0 if which == "v0" else 512
                    tt("dve", vc_t[:, i, vo:vo + 512], ps[bk][:, :], bbb[:, 512 + vo:512 + vo + 512], ALU.add,
                       r=[("ps", bk), "bbb"], w=["vc_t"])
        kcs = alloc(2 * 2 * 512, BF16).rearrange("p (d c n) -> p d c n", d=2, c=2)
        for i in range(2):
            for h in range(4):
                for d_ in range(2):
                    ts("dve", kcs[:, d_, i, h * 128:(h + 1) * 128], kc_t[:, i, h * 128:(h + 1) * 128],
                       DK[:, 4 * d_ + h:4 * d_ + h + 1], kscale, ALU.mult, ALU.mult, r=["kc_t", "DK"], w=["kcs"])
        s0v = s0.rearrange("p (d h n) -> p d h n", d=2, h=4)
        for h in range(4):
            for d_ in range(2):
                first, second = (0, 1) if d_ == 0 else (1, 0)
                bk = nb()
                op("pe", mmgroup([(ps[bk][:, 0:256], kcs[:, d_, first, h * 128:(h + 1) * 128], vc_t[:, first, h * 256:(h + 1) * 256], True, True),
                                  (ps[bk][:, 256:512], kcs[:, d_, second, h * 128:(h + 1) * 128], vc_t[:, second, h * 256:(h + 1) * 256], True, True)]),
                   r=["kcs", "vc_t"], w=[("ps", bk)])
                cp("act", tmpf[:, 0:256], ps[bk][:, 256:512], r=[("ps", bk)], w=["tmpf"])
                stt("dve", s0v[:, d_, h, :], ps[bk][:, 0:256], G128[:, 4 * d_ + h:4 * d_ + h + 1], tmpf[:, 0:256], ALU.mult, ALU.add,
                    r=[("ps", bk), "tmpf", "dec"], w=["s0"])

        phase_reset()
        A1 = alloc(D, F32)
        B1 = alloc(D, F32)
        dma("sp", B1, mod_d[:, 0:1024], w=["modL"])
        dma("sp", A1, mod_d[:, 1024:2048], w=["modL"])
        w_in = alloc(8 * INW, BF16).rearrange("p (k n) -> p k n", k=8)
        for kc in range(8):
            dma("pool", w_in[:, kc, :], w_in_v[:, kc, :], w=["w_in"])
        bfm = alloc(52, F32)
        dma("sp", bfm, b_in_fm_d, w=["bfm"])
        bbb = alloc(2560, F32)
        dma("sp", bbb, b_in_b_d, w=["bbb"])
        xt = [alloc(D, F32), alloc(D, F32)]
        hb4 = [alloc(D, BF16) for _ in range(4)]
        hT = alloc(8 * 512, BF16).rearrange("p (k t) -> p k t", k=8)
        ssq = alloc(4, F32)
        junk = alloc(D, BF16)
        ropet1 = alloc(1024, F32)
        ropet = [ropet1, ropet1]
        qkf = [alloc(512, F32) for _ in range(3)]
        qkr = [alloc(512, F32) for _ in range(3)]
        qko = [alloc(512, BF16) for _ in range(3)]
        ktm = alloc(4 * 512, BF16).rearrange("p (t n) -> p t n", t=4)
        outb = [alloc(512, BF16) for _ in range(3)]
        outf = [alloc(512, F32), alloc(512, F32)]
        sgm1 = alloc(512, F32)
        sgm = [sgm1, sgm1]
        permf = ca("permf")
        cnt = [0]
        bgjobs = []
        for e_ in range(32):
            bgjobs.append((ew1b_d[e_ * 128:(e_ + 1) * 128, :], ew1_d[e_ * 128:(e_ + 1) * 128, :]))
            bgjobs.append((ew3b_d[e_ * 128:(e_ + 1) * 128, :], ew3_d[e_ * 128:(e_ + 1) * 128, :]))
            bgjobs.append((ew2b_d[e_ * 512:(e_ + 1) * 512, :].rearrange("(p a) n -> p (a n)", p=128),
                           ew2_d[e_ * 512:(e_ + 1) * 512, :].rearrange("(p a) n -> p (a n)", p=128)))

        bgpos = [0]

        def bg_issue(n, rtags):
            for (o_, i_) in bgjobs[bgpos[0]:bgpos[0] + n]:
                S.add("pool", lambda e, o_=o_, i_=i_: e.dma_start(out=o_, in_=i_), r=rtags, dma=True, q="bg", bg=True)
            bgpos[0] += n

        def normA(g):
            for ti in range(4):
                i = g * 4 + ti
                b = i % 2
                r0 = g * 512 + ti * 128
                dma("sp", xt[b], x_d[r0:r0 + 128, :], w=[("xt", b)])
                sscol = ssq[:, b:b + 1]
                act(junk, xt[b], AF.Square, r=[("xt", b)], w=["junk", ("ssq", b)], accum=sscol)
                rstd_from_ss(sscol, D, ("ssq", b))
                stt("dve", xt[b], xt[b], sscol, A1, ALU.mult, ALU.mult, r=[("xt", b), ("ssq", b), "modL"], w=[("xt", b)])
                tt("dve", hb4[ti], xt[b], B1, ALU.add, r=[("xt", b), "modL"], w=[("hb4", ti)])

        def normB(g):
            for ti in range(4):
                bk = nb()
                op("pe", trgroup([(psb[bk][:, kc * 128:(kc + 1) * 128], hb4[ti][:, kc * 128:(kc + 1) * 128]) for kc in range(8)], identb),
                   r=[("hb4", ti), "cB"], w=[("ps", bk)])
                cp("act", hT[:, :, ti * 128:(ti + 1) * 128], psb[bk][:, :].rearrange("p (k t) -> p k t", k=8),
                   r=[("ps", bk)], w=["hT"])
        normA(0)
        normB(0)
        for g in range(8):
            t0 = g * 512
            rb = 0
            dma("sp", ropet[rb][:, 0:512], rope_d[:, g * 512:g * 512 + 512], w=[("ropet", rb)])
            dma("sp", ropet[rb][:, 512:1024], rope_d[:, L + g * 512:L + g * 512 + 512], w=[("ropet", rb)])
            bg_issue(5, ["hT"])
            if g + 1 < 8:
                normA(g + 1)

            def qk_mm(cc):
                s3 = cc % 3
                bk = nb()
                op("pe", mmgroup([(ps[bk][:, :], w_in[:, kc, cc * 128:(cc + 1) * 128], hT[:, kc, :], kc == 0, kc == 7)
                                  for kc in range(8)]), r=["hT", "w_in"], w=[("ps", bk)])
                act(qkf[s3], ps[bk][:, :], AF.Identity, r=[("ps", bk), "bfm"], w=[("qkf", s3)], bias=bfm[:, cc:cc + 1])

            def qk_rope(cc):
                s3 = cc % 3
                bk2 = nb()
                op("pe", mm(ps[bk2][:, :], permf, qkf[s3], True, True), r=[("qkf", s3), "cA"], w=[("ps", bk2)])
                tt("dve", qkr[s3], ps[bk2][:, :], ropet[rb][:, 512:1024], ALU.mult, r=[("ps", bk2), ("ropet", rb)], w=[("qkr", s3)])
                tt("dve", qkf[s3], qkf[s3], ropet[rb][:, 0:512], ALU.mult, r=[("qkf", s3), ("ropet", rb)], w=[("qkf", s3)])
                if cc < 4:
                    tt("dve", qko[s3], qkf[s3], qkr[s3], ALU.add, r=[("qkf", s3), ("qkr", s3)], w=[("qko", s3)])
                    dma("sp", qT_d[cc, :, t0:t0 + 512], qko[s3], r=[("qko", s3)])
                else:
                    h = cc - 4
                    tt("dve", qkr[s3], qkf[s3], qkr[s3], ALU.add, r=[("qkf", s3), ("qkr", s3)], w=[("qkr", s3)])
                    act(qko[s3], qkr[s3], AF.Copy, r=[("qkr", s3)], w=[("qko", s3)], scale=kscale)
                    dma("sp", kT_d[h, :, t0:t0 + 512], qko[s3], r=[("qko", s3)])
                    bk3 = nb()
                    op("pe", trgroup([(psb[bk3][:, ti * 128:(ti + 1) * 128], qko[s3][:, ti * 128:(ti + 1) * 128]) for ti in range(4)], identb),
                       r=[("qko", s3), "cB"], w=[("ps", bk3)])
                    cp("act", ktm[:, :, h * 128:(h + 1) * 128], psb[bk3][:, 0:512].rearrange("p (t n) -> p t n", t=4),
                       r=[("ps", bk3)], w=["ktm"])
            for cc in range(8):
                qk_mm(cc)
                if cc >= 1:
                    qk_rope(cc - 1)
            pend = [7]
            for ti in range(4):
                for nh in range(4):
                    c0 = 1024 + nh * 512
                    bk = nb()
                    op("pe", mmgroup([(ps[bk][:, :], hT[:, kc, ti * 128:(ti + 1) * 128], w_in[:, kc, c0:c0 + 512], kc == 0, kc == 7)
                                      for kc in range(8)]), r=["hT", "w_in"], w=[("ps", bk)])
                    if pend:
                        qk_rope(pend.pop())
                        dma("sp", kt_d[t0:t0 + 512, :].rearrange("(t p) n -> p t n", p=128), ktm, r=["ktm"])
                    o3 = cnt[0] % 3
                    ob = outb[o3]
                    otag = ("outb", o3)
                    cnt[0] += 1
                    r0 = t0 + ti * 128
                    if nh < 2:
                        tt("dve", ob, ps[bk][:, :], bbb[:, 512 + nh * 512:1024 + nh * 512], ALU.add, r=[("ps", bk), "bbb"], w=[otag])
                        dma("sp", v_d[r0:r0 + 128, nh * 512:(nh + 1) * 512], ob, r=[otag])
                    else:
                        f2 = nh % 2
                        tt("dve", outf[f2], ps[bk][:, :], bbb[:, 512 + nh * 512:1024 + nh * 512], ALU.add, r=[("ps", bk), "bbb"], w=[("outf", f2)])
                        act(sgm[f2], outf[f2], AF.Sigmoid, r=[("outf", f2)], w=[("sgm", 0)])
                        tt("dve", ob, outf[f2], sgm[f2], ALU.mult, r=[("outf", f2), ("sgm", 0)], w=[otag])
                        dma("sp", gs_d[r0:r0 + 128, (nh - 2) * 512:(nh - 1) * 512], ob, r=[otag])
            for cc in range(28):
                c0 = 3072 + cc * 128
                bk = nb()
                op("pe", mmgroup([(ps[bk][:, :], w_in[:, kc, c0:c0 + 128], hT[:, kc, :], kc == 0, kc == 7)
                                  for kc in range(8)]), r=["hT", "w_in"], w=[("ps", bk)])
                o3 = cnt[0] % 3
                ob = outb[o3]
                otag = ("outb", o3)
                cnt[0] += 1
                act(ob, ps[bk][:, :], AF.Identity if cc < 12 else AF.Sigmoid, r=[("ps", bk), "bfm"], w=[otag],
                    bias=bfm[:, 24 + cc:25 + cc])
                if cc < 12:
                    dma("sp", u_d[cc * 128:(cc + 1) * 128, t0:t0 + 512], ob, r=[otag])
                else:
                    dma("sp", gates_d[(cc - 12) * 128:(cc - 11) * 128, t0:t0 + 512], ob, r=[otag])
            if g + 1 < 8:
                normB(g + 1)

        phase_reset()
        qTt2 = [alloc(L, BF16) for _ in range(2)]
        kTt2 = [alloc(L, BF16) for _ in range(2)]
        qfb2 = [alloc(2 * L, BF16).rearrange("p (d t) -> p d t", d=2) for _ in range(2)]
        ktk1 = alloc(32 * 128, BF16).rearrange("p (c n) -> p c n", c=32)
        kfb2 = [alloc(2 * 32 * 128, BF16).rearrange("p (d c n) -> p d c n", d=2, c=32) for _ in range(2)]
        vt2 = [alloc(32 * 256, BF16).rearrange("p (c n) -> p c n", c=32) for _ in range(2)]
        SB = alloc(32 * 256, BF16).rearrange("p (c n) -> p c n", c=32)
        SF = alloc(32 * 256, BF16).rearrange("p (c n) -> p c n", c=32)
        SstF = [alloc(256, F32), alloc(256, F32)]
        SstB = [alloc(256, F32), alloc(256, F32)]
        PT = [alloc(128, BF16) for _ in range(3)]
        gst = [alloc(256, BF16) for _ in range(3)]
        rgo = [alloc(256, BF16) for _ in range(3)]
        rss = alloc(4, F32)
        junk = alloc(256, BF16)
        def loadpre(h):
            hb_ = h % 2
            qTt, kTt, qfb, kfb, vt, ktk = qTt2[hb_], kTt2[hb_], qfb2[hb_], kfb2[hb_], vt2[hb_], ktk1
            dma("sp", qTt, qT_d[h], w=[("qTt", hb_)])
            dma("sp", kTt, kT_d[h], w=[("kTt", hb_)])
            dma("sp", ktk, kt_d[:, h * 128:(h + 1) * 128].rearrange("(c p) n -> p c n", p=128), w=["ktk"])
            dma("sp", vt, v_d[:, h * 256:(h + 1) * 256].rearrange("(c p) n -> p c n", p=128), w=[("vt", hb_)])
            for d_ in range(2):
                tt("dve", qfb[:, d_, :].rearrange("p (c i) -> p c i", i=128), qTt.rearrange("p (c i) -> p c i", i=128),
                   DQf[:, 4 * d_ + h, :].unsqueeze(1).to_broadcast([128, 32, 128]), ALU.mult, r=[("qTt", hb_), "DQ"], w=[("qfb", hb_, d_)])
                act(kfb[:, d_].rearrange("p c n -> p (c n)"), ktk.rearrange("p c n -> p (c n)"), AF.Copy, r=["ktk", "DK"], w=[("kfb", hb_, d_)],
                    scale=DK[:, 4 * d_ + h:4 * d_ + h + 1])
        loadpre(0)
        for h in range(4):
            hb_ = h % 2
            qTt, kTt, qfb, kfb, vt = qTt2[hb_], kTt2[hb_], qfb2[hb_], kfb2[hb_], vt2[hb_]
            cp("dve", SstF[0], s0v[:, 0, h, :], r=["s0"], w=[("SstF", 0)])
            cp("act", SF[:, 0, :], s0v[:, 0, h, :], r=["s0"], w=[("SF", 0)])
            cp("dve", SstB[0], s0v[:, 1, h, :], r=["s0"], w=[("SstB", 0)])
            cp("act", SB[:, 31, :], s0v[:, 1, h, :], r=["s0"], w=[("SB", 31)])
            for i in range(31):
                cur, nxt = i % 2, (i + 1) % 2
                cf, cbk = i, 31 - i
                bk = nb()
                op("pe", mmgroup([(ps[bk][:, 0:256], kfb[:, 0, cf, :], vt[:, cf, :], True, True),
                                  (ps[bk][:, 256:512], kfb[:, 1, cbk, :], vt[:, cbk, :], True, True)]),
                   r=[("kfb", hb_, 0), ("kfb", hb_, 1), ("vt", hb_)], w=[("ps", bk)])
                stt("dve", SstF[nxt], SstF[cur], G128[:, h:h + 1], ps[bk][:, 0:256], ALU.mult, ALU.add,
                    r=[("SstF", cur), ("ps", bk), "dec"], w=[("SstF", nxt)])
                stt("dve", SstB[nxt], SstB[cur], G128[:, 4 + h:5 + h], ps[bk][:, 256:512], ALU.mult, ALU.add,
                    r=[("SstB", cur), ("ps", bk), "dec"], w=[("SstB", nxt)])
                cp("act", SF[:, cf + 1, :], SstF[nxt], r=[("SstF", nxt)], w=[("SF", cf + 1)])
                cp("act", SB[:, cbk - 1, :], SstB[nxt], r=[("SstB", nxt)], w=[("SB", cbk - 1)])

            if h + 1 < 4:
                loadpre(h + 1)

            def scores(c):
                b3 = c % 3
                bk = nb()
                op("pe", mm(ps[bk][:, 0:128], kTt[:, c * 128:(c + 1) * 128], qTt[:, c * 128:(c + 1) * 128], True, True),
                   r=[("kTt", hb_), ("qTt", hb_)], w=[("ps", bk)])
                tt("dve", PT[b3], ps[bk][:, 0:128], Dm[:, h, :], ALU.mult, r=[("ps", bk), "Dm"], w=[("PT", b3)])
                dma("sp", gst[b3], gs_d[c * 128:(c + 1) * 128, h * 256:(h + 1) * 256], w=[("gst", b3)])

            def outc(c):
                b3 = c % 3
                bo = nb()
                op("pe", mmgroup([(ps[bo][:, 0:256], PT[b3], vt[:, c, :], True, False),
                                  (ps[bo][:, 0:256], qfb[:, 0, c * 128:(c + 1) * 128], SF[:, c, :], False, False),
                                  (ps[bo][:, 0:256], qfb[:, 1, c * 128:(c + 1) * 128], SB[:, c, :], False, True)]),
                   r=[("PT", b3), ("vt", hb_), ("qfb", hb_, 0), ("qfb", hb_, 1), ("SF", c), ("SB", c)], w=[("ps", bo)])
                sscol = rss[:, b3:b3 + 1]
                act(junk, ps[bo][:, 0:256], AF.Square, r=[("ps", bo)], w=["junk", ("rss", b3)], accum=sscol)
                rstd_from_ss(sscol, 256, ("rss", b3))
                stt("dve", rgo[b3], ps[bo][:, 0:256], sscol, gst[b3], ALU.mult, ALU.mult,
                    r=[("ps", bo), ("rss", b3), ("gst", b3)], w=[("rgo", b3)])
                dma("sp", rg_d[c * 128:(c + 1) * 128, h * 256:(h + 1) * 256], rgo[b3], r=[("rgo", b3)])
                if c % 3 == 0 and c < 30:
                    bg_issue(1, [("rgo", b3)])
            scores(0)
            scores(1)
            for c in range(32):
                if c + 2 < 32:
                    scores(c + 2)
                outc(c)

        phase_reset()
        fT = alloc(L, F32)
        dma("sp", fT[0:33, :], featsT_d, w=["fT"])
        hw1 = alloc(64, F32)
        dma("sp", hw1[0:33, :], hw1_d, w=["hw1"])
        hvec = alloc(4, F32)
        dma("sp", hvec[0:64, :], hvec_d, w=["hvec"])
        hw2 = alloc(64, F32)
        dma("sp", hw2[0:64, :], hw2_d, w=["hw2"])
        hw3 = alloc(1024, F32)
        dma("sp", hw3[0:64, :], hw3_d, w=["hw3"])
        tv = alloc(L, F32)
        dma("sp", tv, tvec_d, w=["tv"])
        fb = alloc(2, F32)
        tt("dve", fb[0:64, 0:1], hvec[0:64, 0:1], hvec[0:64, 1:2], ALU.mult, r=["hvec"], w=["fb"])
        tt("dve", fb[0:64, 1:2], hvec[0:64, 2:3], hvec[0:64, 1:2], ALU.mult, r=["hvec"], w=["fb"])
        hid = [alloc(L, F32), alloc(L, F32)]
        argt = alloc(512, F32)
        kti = alloc(512, I32)
        ktf = alloc(512, F32)
        TWO_PI = 2.0 * math.pi

        def sin_layer(dst, bank, bcol):
            act(argt[0:64, :], ps[bank][0:64, :], AF.Identity, r=[("ps", bank), "hvec", "fb"], w=["argt"],
                scale=hvec[0:64, 1:2], bias=fb[0:64, bcol:bcol + 1])
            ts("dve", ktf[0:64, :], argt[0:64, :], 1.0 / TWO_PI, None, ALU.mult, None, r=["argt"], w=["ktf"])
            cp("dve", kti[0:64, :], ktf[0:64, :], r=["ktf"], w=["kti"])
            cp("dve", ktf[0:64, :], kti[0:64, :], r=["kti"], w=["ktf"])
            stt("dve", argt[0:64, :], ktf[0:64, :], -TWO_PI, argt[0:64, :], ALU.mult, ALU.add, r=["ktf", "argt"], w=["argt"])
            act(dst, argt[0:64, :], AF.Sin, r=["argt"], w=["hid"])
        for tg in range(8):
            sl = slice(tg * 512, (tg + 1) * 512)
            bk = nb()
            op("pe", mm(ps[bk][0:64, :], hw1[0:33, :], fT[0:33, sl], True, True), r=["hw1", "fT"], w=[("ps", bk)])
            sin_layer(hid[0][0:64, sl], bk, 0)
        for tg in range(8):
            sl = slice(tg * 512, (tg + 1) * 512)
            bk = nb()
            op("pe", mm(ps[bk][0:64, :], hw2[0:64, :], hid[0][0:64, sl], True, True), r=["hw2", "hid"], w=[("ps", bk)])
            sin_layer(hid[1][0:64, sl], bk, 1)
        S.barrier()
        fl = [alloc(L, F32), alloc(L, F32)]
        l1 = alloc(8, F32)
        win = alloc(512, F32)
        nd = ca("negdelta")
        win = [win, alloc(512, F32)]
        hsb = [alloc(L, BF16), alloc(L, BF16)]
        for cp_ in range(4):
            for b2 in range(2):
                cc = cp_ + 4 * b2
                for tg in range(8):
                    sl = slice(tg * 512, (tg + 1) * 512)
                    bk = nb()
                    w2_ = tg % 2
                    op("pe", mm(ps[bk][:, :], hw3[0:64, cc * 128:(cc + 1) * 128], hid[1][0:64, sl], True, True), r=["hw3"], w=[("ps", bk)])
                    act(win[w2_], tv[:, sl], AF.Exp, r=["tv", "cA"], w=[("win", w2_)], scale=nd[:, cc:cc + 1])
                    tt("dve", fl[b2][:, sl], ps[bk][:, :], win[w2_], ALU.mult, r=[("ps", bk), ("win", w2_)], w=[("fl", b2)])
                op("dve", lambda e, b2=b2, cc=cc: e.tensor_reduce(out=l1[:, cc:cc + 1], in_=fl[b2], axis=AX.X, op=ALU.add, apply_absolute_value=True),
                   r=[("fl", b2)], w=["l1"])
                op("dve", lambda e, cc=cc: e.reciprocal(out=l1[:, cc:cc + 1], in_=l1[:, cc:cc + 1]), r=["l1"], w=["l1"])
            act(fl[1], fl[1], AF.Copy, r=[("fl", 1), "l1"], w=[("fl", 1)], scale=l1[:, cp_ + 4:cp_ + 5])
            stt("dve", hsb[0], fl[0], l1[:, cp_:cp_ + 1], fl[1], ALU.mult, ALU.add, r=[("fl", 0), ("fl", 1), "l1"], w=[("hsb", 0)])
            stt("dve", hsb[1], fl[0], l1[:, cp_:cp_ + 1], fl[1], ALU.mult, ALU.subtract, r=[("fl", 0), ("fl", 1), "l1"], w=[("hsb", 1)])
            dma("sp", hs_d[0, cp_ * 128:(cp_ + 1) * 128, :], hsb[0], r=[("hsb", 0)], w=["hs_d"])
            dma("sp", hs_d[1, cp_ * 128:(cp_ + 1) * 128, :], hsb[1], r=[("hsb", 1)], w=["hs_d"])

        E1c, E1s, E1cT, E1sT = cb("E1c"), cb("E1s"), cb("E1cT"), cb("E1sT")
        cdv = cd_d.rearrange("r (k a c) -> r k a c", k=128, a=32)
        cdv2 = cd_d.rearrange("r (q m a c) -> r m a q c", q=32, m=4, a=32)
        FB = {}

        def fft_forward(want, consume):
            sigF, sigJ, C2, ftab, stg = FB["sigF"], FB["sigJ"], FB["C2"], FB["ftab"], FB["stg"]
            sv = sigF.rearrange("p k (j a) -> p k a j", a=32)
            for a2 in range(16):
                bk = nb()
                op("pe", trgroup([(psb[bk][:, (aa * 4 + cc) * 128:(aa * 4 + cc + 1) * 128], sv[:, cc, a2 * 2 + aa, :])
                                  for aa in range(2) for cc in range(4)], identb), r=["sigF", "cB"], w=[("ps", bk)])
                cp("act" if a2 % 2 == 0 else "dve", sigJ[:, a2 * 2:a2 * 2 + 2, :], psb[bk][:, :].rearrange("p (a c) -> p a c", a=2),
                   r=[("ps", bk)], w=["sigJ"])
            for a in range(32):
                for ri in range(2):
                    bk = nb()
                    op("pe", mm(ps[bk][:, :], E1c if ri == 0 else E1s, sigJ[:, a, :], True, True), r=["sigJ", "cB"], w=[("ps", bk)])
                    cp("act" if ri == 0 else "dve", C2[ri][:, a, :], ps[bk][:, :], r=[("ps", bk)], w=[("C2", ri)])
            for ri in range(2):
                dma("sp", cdv[ri], C2[ri], r=[("C2", ri)], w=[("cd", ri)])
            for ri in range(2):
                for m in range(4):
                    dma("sp", C2[ri][m * 32:(m + 1) * 32, :, :], cdv2[ri, m], r=[("cd", ri)], w=[("C2", ri)])
            for q in range(32):
                bre = bim = None
                if want in ("re", "both"):
                    bre = nb()
                    op("pe", mmgroup([(ps[bre][:, :], ftab[:, 0, q, :], C2[0][:, q, :], True, False),
                                      (ps[bre][:, :], ftab[:, 2, q, :], C2[1][:, q, :], False, True)]),
                       r=["ftab", ("C2", 0), ("C2", 1)], w=[("ps", bre)])
                if want in ("im", "both"):
                    bim = nb()
                    op("pe", mmgroup([(ps[bim][:, :], ftab[:, 1, q, :], C2[0][:, q, :], True, False),
                                      (ps[bim][:, :], ftab[:, 0, q, :], C2[1][:, q, :], False, True)]),
                       r=["ftab", ("C2", 0), ("C2", 1)], w=[("ps", bim)])
                consume(q, bre, bim)

        phase_reset()
        C2 = [alloc(32 * 512, BF16).rearrange("p (q c) -> p q c", q=32) for _ in range(2)]
        ftab = alloc(3 * 32 * 128, BF16).rearrange("p (y q m) -> p y q m", y=3, q=32)
        dma("sp", ftab, ftab_d.rearrange("p (y q m) -> p y q m", y=3, q=32), w=["ftab"])
        sigF = alloc(4 * L, BF16).rearrange("p (k t) -> p k t", k=4)
        sigJ = alloc(32 * 512, BF16).rearrange("p (a c) -> p a c", a=32)
        FB.update(sigF=sigF, sigJ=sigJ, C2=C2, ftab=ftab, stg=None)
        hstage = [alloc(2048, F32), alloc(2048, F32)]
        for which in range(2):
            dma("sp", sigF, hs_d[which].rearrange("(k p) t -> p k t", p=128), r=["hs_d"], w=["sigF"])

            def consume_h(q, bre, bim, which=which):
                bk = bre if which == 0 else bim
                hi_ = (q // 4) % 2
                q4 = q % 4
                hs = hstage[hi_]
                tg_ = ("hstage", hi_)
                cp("act", hs[:, q4 * 512:(q4 + 1) * 512], ps[bk][:, :], r=[("ps", bk)], w=[tg_])
                if q4 == 3:
                    dma("sp", hd_d[which, :, (q - 3) * 512:(q + 1) * 512], hs, r=[tg_], w=["hd"])
                    bg_issue(1, [tg_])
            fft_forward("re" if which == 0 else "im", consume_h)

        phase_reset()
        C2 = [alloc(32 * 512, BF16).rearrange("p (q c) -> p q c", q=32) for _ in range(2)]
        mark0 = st["off"]
        ftab = alloc(3 * 32 * 128, BF16).rearrange("p (y q m) -> p y q m", y=3, q=32)
        dma("sp", ftab, ftab_d.rearrange("p (y q m) -> p y q m", y=3, q=32), w=["ftab"])
        sigF = alloc(4 * L, BF16).rearrange("p (k t) -> p k t", k=4)
        hyc = alloc(60, F32).rearrange("p (c k) -> p c k", c=12)
        dma("sp", hyc, hyc_d.rearrange("p (c k) -> p c k", c=12), w=["hyc"])
        hsk = alloc(4, F32)
        dma("sp", hsk, hyskip_d, w=["hsk"])
        mark1 = st["off"]
        ub = alloc(L + 2, BF16)
        cacc = alloc(L, F32)
        x1c = alloc(L, F32)

        def conv_chunk(cc, dst, ub, cacc):
            dma("sp", ub[:, 1:L + 1], u_d[cc * 128:(cc + 1) * 128, :], w=["ub"])
            ts("dve", cacc, ub[:, 0:L], hyc[:, cc, 0:1], hyc[:, cc, 3:4], ALU.mult, ALU.add, r=["ub", "hyc"], w=["cacc"])
            stt("dve", cacc, ub[:, 1:L + 1], hyc[:, cc, 1:2], cacc, ALU.mult, ALU.add, r=["ub", "hyc", "cacc"], w=["cacc"])
            stt("dve", dst, ub[:, 2:L + 2], hyc[:, cc, 2:3], cacc, ALU.mult, ALU.add, r=["ub", "hyc", "cacc"], w=["convout"])
        op("dve", lambda e: e.memset(ub, 0.0), w=["ub"])
        for cc in range(4):
            conv_chunk(4 + cc, x1c, ub, cacc)
            conv_chunk(8 + cc, cacc, ub, cacc)
            tt("dve", sigF[:, cc, :], cacc, x1c, ALU.mult, r=["convout"], w=["sigF", "convout"])
            dma("sp", z_d[cc * 128:(cc + 1) * 128, :], sigF[:, cc, :], r=["sigF"], w=["z_d"])
        S.barrier()
        st["off"] = mark1
        sigJ = alloc(32 * 512, BF16).rearrange("p (a c) -> p a c", a=32)
        FB.update(sigF=sigF, sigJ=sigJ, C2=C2, ftab=ftab, stg=None)
        Yst = [sigJ, sigF.rearrange("p k t -> p (k t)").rearrange("p (q c) -> p q c", q=32)]
        ytag = ["sigJ", "sigF"]
        hre = [alloc(512, F32), alloc(512, F32)]
        him = [alloc(512, F32), alloc(512, F32)]
        xre2 = [alloc(512, F32), alloc(512, F32)]
        xim2 = [alloc(512, F32), alloc(512, F32)]
        t12 = [alloc(512, F32), alloc(512, F32)]
        t22 = [alloc(512, F32), alloc(512, F32)]

        def consume_z(q, bre, bim):
            b2 = q % 2
            xre, xim, t1, t2 = xre2[b2], xim2[b2], t12[b2], t22[b2]
            dma("sp", hre[b2], hd_d[0, :, q * 512:(q + 1) * 512], w=[("hre", b2)])
            dma("sp", him[b2], hd_d[1, :, q * 512:(q + 1) * 512], w=[("him", b2)])
            cp("act", xre, ps[bre][:, :], r=[("ps", bre)], w=[("xre", b2)])
            cp("act", xim, ps[bim][:, :], r=[("ps", bim)], w=[("xim", b2)])
            tt("dve", t1, xre, hre[b2], ALU.mult, r=[("xre", b2), ("hre", b2)], w=[("t1", b2)])
            tt("dve", t2, xim, him[b2], ALU.mult, r=[("xim", b2), ("him", b2)], w=[("t2", b2)])
            tt("dve", Yst[0][:, q, :], t1, t2, ALU.subtract, r=[("t1", b2), ("t2", b2)], w=[ytag[0]])
            tt("dve", t1, xre, him[b2], ALU.mult, r=[("xre", b2), ("him", b2)], w=[("t1", b2)])
            tt("dve", t2, xim, hre[b2], ALU.mult, r=[("xim", b2), ("hre", b2)], w=[("t2", b2)])
            tt("dve", Yst[1][:, q, :], t1, t2, ALU.add, r=[("t1", b2), ("t2", b2)], w=[ytag[1]])
        fft_forward("both", consume_z)
        itab = ftab
        dma("sp", itab, itab_d.rearrange("p (y q m) -> p y q m", y=3, q=32), w=["ftab"])
        for q in range(32):
            for ri in range(2):
                bk = nb()
                if ri == 0:
                    lst = [(ps[bk][:, :], itab[:, 0, q, :], Yst[0][:, q, :], True, False), (ps[bk][:, :], itab[:, 1, q, :], Yst[1][:, q, :], False, True)]
                else:
                    lst = [(ps[bk][:, :], itab[:, 0, q, :], Yst[1][:, q, :], True, False), (ps[bk][:, :], itab[:, 2, q, :], Yst[0][:, q, :], False, True)]
                op("pe", mmgroup(lst), r=["ftab", ytag[0], ytag[1]], w=[("ps", bk)])
                cp("act" if ri == 0 else "dve", C2[ri][:, q, :], ps[bk][:, :], r=[("ps", bk)], w=[("C2", ri)])
        Dst = [c_.rearrange("p q c -> p (q c)").rearrange("p (a c) -> p a c", a=32) for c_ in C2]
        for ri in range(2):
            for m in range(4):
                dma("sp", cdv2[ri, m], C2[ri][m * 32:(m + 1) * 32, :, :], r=[("C2", ri)], w=[("cd", ri)])
        for ri in range(2):
            dma("sp", Dst[ri], cdv[ri], r=[("cd", ri)], w=[("C2", ri)])
        S.barrier()
        st["off"] = mark0
        ub = alloc(L + 2, BF16)
        cacc = alloc(L, F32)
        hyo = alloc(L, BF16)
        ycv = alloc(L, F32)
        zr = alloc(L, BF16)
        hyc = alloc(60, F32).rearrange("p (c k) -> p c k", c=12)
        dma("sp", hyc, hyc_d.rearrange("p (c k) -> p c k", c=12), w=["hyc"])
        hsk = alloc(4, F32)
        dma("sp", hsk, hyskip_d, w=["hsk"])
        op("dve", lambda e: e.memset(ub, 0.0), w=["ub"])
        for cc in range(4):
            yv = ycv.rearrange("p (j a) -> p a j", a=32)
            for a4 in range(8):
                bk = nb()
                lst = []
                for aa in range(4):
                    a = a4 * 4 + aa
                    lst.append((ps[bk][:, aa * 128:(aa + 1) * 128], Dst[0][:, a, cc * 128:(cc + 1) * 128], E1cT, True, False))
                    lst.append((ps[bk][:, aa * 128:(aa + 1) * 128], Dst[1][:, a, cc * 128:(cc + 1) * 128], E1sT, False, True))
                op("pe", mmgroup(lst), r=[("C2", 0), ("C2", 1), "cB"], w=[("ps", bk)])
                act(yv[:, a4 * 4:a4 * 4 + 4, :], ps[bk][:, :].rearrange("p (a j) -> p a j", a=4), AF.Copy,
                    r=[("ps", bk)], w=["ycv"], scale=1.0 / 4096.0)
            dma("sp", zr, z_d[cc * 128:(cc + 1) * 128, :], w=["zr"])
            stt("dve", ycv, zr, hsk[:, cc:cc + 1], ycv, ALU.mult, ALU.add, r=["zr", "hsk", "ycv"], w=["ycv"])
            conv_chunk(cc, cacc, ub, cacc)
            tt("dve", hyo, ycv, cacc, ALU.mult, r=["ycv", "convout"], w=["hyo", "convout"])
            dma("sp", hy_d[cc * 128:(cc + 1) * 128, :], hyo, r=["hyo"])

        phase_reset()
        h2all = alloc(NT * D, BF16).rearrange("p (t n) -> p t n", t=NT)
        oh_all = alloc(NT * 64, BF16).rearrange("p (t k e) -> p t k e", t=NT, k=2)
        rk_all = alloc(NT * 2, F32).rearrange("p (t k) -> p t k", t=NT)
        wtsv = wts.rearrange("p (t k) -> p t k", t=NT)
        destv = dest.rearrange("p (t k) -> p t k", t=NT)
        base = alloc(32, F32)
        op("dve", lambda e: e.memset(base, 0.0), w=["base"])
        mark = st["off"]
        rwo = alloc(8 * D, BF16).rearrange("p (k n) -> p k n", k=8)
        hwo = alloc(4 * D, BF16).rearrange("p (k n) -> p k n", k=4)
        wo = alloc(8 * D, BF16).rearrange("p (k n) -> p k n", k=8)
        dma("pool", rwo, ret_w_o_d.rearrange("(k p) n -> p k n", p=128), w=["rwo"])
        dma("pool", hwo, hy_w_o_d.rearrange("(k p) n -> p k n", p=128), w=["hwo"])
        dma("pool", wo, w_out_d.rearrange("(k p) n -> p k n", p=128), w=["wo"])
        wr = alloc(8 * 36, F32).rearrange("p (k n) -> p k n", k=8)
        dma("sp", wr, wr_d.rearrange("(k p) n -> p k n", p=128), w=["wr"])
        brb = alloc(36, F32)
        dma("sp", brb, br_d, w=["brb"])
        rgt = [alloc(D, BF16), alloc(D, BF16)]
        rgT = alloc(8 * 512, BF16).rearrange("p (k t) -> p k t", k=8)
        hyT = alloc(4 * 512, BF16).rearrange("p (k t) -> p k t", k=4)
        gt_ = [alloc(512, BF16), alloc(512, BF16)]
        gh_ = [alloc(512, BF16), alloc(512, BF16)]
        m1 = alloc(512, F32)
        m2 = alloc(512, F32)
        mixT = alloc(8 * 512, BF16).rearrange("p (k t) -> p k t", k=8)
        x2t = [alloc(D, F32), alloc(D, F32)]
        GT1 = alloc(D, F32)
        A2 = alloc(D, F32)
        B2 = alloc(D, F32)
        dma("sp", GT1, mod_d[:, 2048:3072], w=["modL"])
        dma("sp", B2, mod_d[:, 3072:4096], w=["modL"])
        dma("sp", A2, mod_d[:, 4096:5120], w=["modL"])
        h2f = alloc(D, F32)
        h2T = alloc(8 * 128, F32).rearrange("p (k t) -> p k t", k=8)
        ss2 = alloc(2, F32)
        lg = alloc(36, F32)
        sm = alloc(64, F32)
        elm = alloc(32, F32)
        top8 = alloc(8, F32)
        ohb = alloc(32, BF16)
        posn = alloc(32, F32)
        junk2 = alloc(D, BF16)
        tri = cb("tri")
        onesb = cb("onesb")
        h2f2 = [h2f, alloc(D, F32)]
        h2T2 = [h2T, alloc(8 * 128, F32).rearrange("p (k t) -> p k t", k=8)]
        ohb2 = [ohb, alloc(32, BF16)]
        sm4 = alloc(32, F32)

        def T1(tile_i, ti):
            b = tile_i % 2
            r0 = tile_i * 128
            hf_ = h2f2[b]
            dma("sp", x2t[b], x_d[r0:r0 + 128, :], w=[("x2t", b)])
            for nh in range(2):
                bk = nb()
                op("pe", mmgroup([(ps[bk][:, :], mixT[:, kc, ti * 128:(ti + 1) * 128], wo[:, kc, nh * 512:(nh + 1) * 512], kc == 0, kc == 7)
                                  for kc in range(8)]), r=["mixT", "wo"], w=[("ps", bk)])
                sl = slice(nh * 512, (nh + 1) * 512)
                tt("dve", m1, ps[bk][:, :], GT1[:, sl], ALU.mult, r=[("ps", bk), "modL"], w=["m1"])
                tt("dve", x2t[b][:, sl], x2t[b][:, sl], m1, ALU.add, r=[("x2t", b), "m1"], w=[("x2t", b)])
            dma("sp", x2_d[r0:r0 + 128, :], x2t[b], r=[("x2t", b)])
            sscol = ss2[:, b:b + 1]
            act(junk2, x2t[b], AF.Square, r=[("x2t", b)], w=["junk2", ("ss2", b)], accum=sscol)
            rstd_from_ss(sscol, D, ("ss2", b))
            stt("dve", hf_, x2t[b], sscol, A2, ALU.mult, ALU.mult, r=[("x2t", b), ("ss2", b), "modL"], w=[("h2f", b)])
            tt("dve", hf_, hf_, B2, ALU.add, r=[("h2f", b), "modL"], w=[("h2f", b)])
            cp("act", h2all[:, tile_i, :], hf_, r=[("h2f", b)], w=[("h2all", tile_i)])

        def T2(tile_i):
            b = tile_i % 2
            hf_ = h2f2[b]
            hT_ = h2T2[b]
            bk0, bk1 = nb(), nb()
            op("pe", trgroup([((ps[bk0] if kc < 4 else ps[bk1])[:, (kc % 4) * 128:(kc % 4 + 1) * 128], hf_[:, kc * 128:(kc + 1) * 128])
                              for kc in range(8)], identf), r=[("h2f", b), "cA"], w=[("ps", bk0), ("ps", bk1)])
            cp("act", hT_[:, 0:4, :], ps[bk0][:, :].rearrange("p (k t) -> p k t", k=4), r=[("ps", bk0)], w=[("h2T", b)])
            cp("act", hT_[:, 4:8, :], ps[bk1][:, :].rearrange("p (k t) -> p k t", k=4), r=[("ps", bk1)], w=[("h2T", b)])

        def T3(tile_i):
            b = tile_i % 2
            hT_ = h2T2[b]
            ohb_ = ohb2[b]
            bl = nb()
            op("pe", mmgroup([(ps[bl][:, 0:36], hT_[:, kc, :], wr[:, kc, :], kc == 0, kc == 7) for kc in range(8)]),
               r=[("h2T", b), "wr"], w=[("ps", bl)])
            tt("dve", lg, ps[bl][:, 0:36], brb, ALU.add, r=[("ps", bl), "brb"], w=["lg"])
            gmax = sm[:, 0:1]
            op("dve", lambda e: e.tensor_reduce(out=sm[:, 0:1], in_=lg[:, 0:4], axis=AX.X, op=ALU.max), r=["lg"], w=["sm"])
            ts("dve", sm[:, 4:8], lg[:, 0:4], gmax, None, ALU.subtract, None, r=["lg", "sm"], w=["sm"])
            act(sm[:, 8:12], sm[:, 4:8], AF.Exp, r=["sm"], w=["sm"], accum=sm[:, 1:2])
            op("dve", lambda e: e.reciprocal(out=sm[:, 2:3], in_=sm[:, 1:2]), r=["sm"], w=["sm"])
            ts("dve", sm[:, 12:16], lg[:, 0:4], gmax, None, ALU.is_equal, None, r=["lg", "sm"], w=["sm"])
            ts("dve", sm[:, 16:20], sm[:, 12:16], 1e30, -1e30, ALU.mult, ALU.add, r=["sm"], w=["sm"])
            tt("dve", elm.rearrange("p (g e) -> p g e", g=4), lg[:, 4:36].rearrange("p (g e) -> p g e", g=4),
               sm[:, 12:16].unsqueeze(2).to_broadcast([128, 4, 8]), ALU.mult, r=["lg", "sm"], w=["elm"])
            tt("dve", elm.rearrange("p (g e) -> p g e", g=4), elm.rearrange("p (g e) -> p g e", g=4),
               sm[:, 16:20].unsqueeze(2).to_broadcast([128, 4, 8]), ALU.add, r=["elm", "sm"], w=["elm"])
            op("dve", lambda e: e.max(out=top8, in_=elm), r=["elm"], w=["top8"])
            tt("dve", sm[:, 20:21], top8[:, 1:2], top8[:, 0:1], ALU.subtract, r=["top8"], w=["sm"])
            act(sm[:, 21:22], sm[:, 20:21], AF.Exp, r=["sm"], w=["sm"])
            ts("dve", sm[:, 21:22], sm[:, 21:22], 1.0, None, ALU.add, None, r=["sm"], w=["sm"])
            op("dve", lambda e: e.reciprocal(out=sm[:, 22:23], in_=sm[:, 21:22]), r=["sm"], w=["sm"])
            ts("dve", sm[:, 23:24], sm[:, 22:23], -1.0, 1.0, ALU.mult, ALU.add, r=["sm"], w=["sm"])
            tt("dve", wtsv[:, tile_i, :], sm[:, 22:24], sm[:, 2:3].to_broadcast([128, 2]), ALU.mult, r=["sm"], w=["wts"])
            for k_ in range(2):
                ts("dve", oh_all[:, tile_i, k_, :], elm, top8[:, k_:k_ + 1], None, ALU.is_equal, None, r=["elm", "top8"], w=[("oh", tile_i)])
            tt("dve", ohb_, oh_all[:, tile_i, 0, :], oh_all[:, tile_i, 1, :], ALU.add, r=[("oh", tile_i)], w=[("ohb", b)])

        def T4(tile_i):
            b = tile_i % 2
            ohb_ = ohb2[b]
            bc = nb()
            op("pe", mmgroup([(ps[bc][:, 0:32], tri, ohb_, True, True), (ps[bc][:, 32:64], onesb, ohb_, True, True)]),
               r=[("ohb", b), "cB"], w=[("ps", bc)])
            tt("dve", posn, ps[bc][:, 0:32], base, ALU.add, r=[("ps", bc), "base"], w=["posn"])
            for k_ in range(2):
                tt("dve", sm4, oh_all[:, tile_i, k_, :], posn, ALU.mult, r=[("oh", tile_i), "posn"], w=["sm4"])
                op("dve", lambda e, k_=k_: e.tensor_reduce(out=rk_all[:, tile_i, k_:k_ + 1], in_=sm4, axis=AX.X, op=ALU.add),
                   r=["sm4"], w=["rk"])
            tt("dve", base, base, ps[bc][:, 32:64], ALU.add, r=[("ps", bc), "base"], w=["base"])

        for g in range(8):
            t0 = g * 512
            for ti in range(4):
                b = ti % 2
                dma("sp", rgt[b], rg_d[t0 + ti * 128:t0 + (ti + 1) * 128, :], w=[("rgt", b)])
                bk = nb()
                op("pe", trgroup([(psb[bk][:, kc * 128:(kc + 1) * 128], rgt[b][:, kc * 128:(kc + 1) * 128]) for kc in range(8)], identb),
                   r=[("rgt", b), "cB"], w=[("ps", bk)])
                cp("act", rgT[:, :, ti * 128:(ti + 1) * 128], psb[bk][:, :].rearrange("p (k t) -> p k t", k=8), r=[("ps", bk)], w=["rgT"])
            dma("sp", hyT, hy_d[:, t0:t0 + 512].rearrange("(k p) t -> p k t", p=128), w=["hyT"])
            for fo in range(8):
                b = fo % 2
                dma("sp", gt_[b], gates_d[fo * 128:(fo + 1) * 128, t0:t0 + 512], w=[("gt", b)])
                dma("sp", gh_[b], gates_d[1024 + fo * 128:1024 + (fo + 1) * 128, t0:t0 + 512], w=[("gh", b)])
                ba = nb()
                op("pe", mmgroup([(ps[ba][:, :], rwo[:, kc, fo * 128:(fo + 1) * 128], rgT[:, kc, :], kc == 0, kc == 7) for kc in range(8)]),
                   r=["rwo", "rgT"], w=[("ps", ba)])
                bb_ = nb()
                op("pe", mmgroup([(ps[bb_][:, :], hwo[:, kc, fo * 128:(fo + 1) * 128], hyT[:, kc, :], kc == 0, kc == 3) for kc in range(4)]),
                   r=["hwo", "hyT"], w=[("ps", bb_)])
                tt("dve", m1, ps[ba][:, :], gt_[b], ALU.mult, r=[("ps", ba), ("gt", b)], w=["m1"])
                tt("dve", m2, ps[bb_][:, :], gh_[b], ALU.mult, r=[("ps", bb_), ("gh", b)], w=["m2"])
                tt("dve", mixT[:, fo, :], m1, m2, ALU.add, r=["m1", "m2"], w=["mixT"])
            for ti in range(4):
                tile_i = g * 4 + ti
                T1(tile_i, ti)
                if tile_i >= 1:
                    T2(tile_i - 1)
                if tile_i >= 2:
                    T3(tile_i - 2)
                if tile_i >= 3:
                    T4(tile_i - 3)
        T2(31)
        T3(30)
        T4(29)
        T3(31)
        T4(30)
        T4(31)

        S.barrier()
        st["off"] = mark
        cRt = alloc(host_consts()["cR"].shape[1], F32)
        dma("sp", cRt, cR_d, w=["cR"])

        def cr(name, shp):
            o, n = CR[name]
            v = cRt[:, o:o + n]
            if shp == 3:
                return v.rearrange("p (a b) -> p a b", b=(16 if name == "thr" else 32))
            return v
        big = alloc(NSLOT * 32, F32)
        nblk = alloc(32, F32)
        pend = alloc(32, F32)
        pstart = alloc(32, F32)
        esl = alloc(NSLOT, F32)
        idxf = alloc(NSLOT * 5, F32).rearrange("p (s k) -> p s k", k=5)
        b3 = big[:, 0:512].rearrange("p (e j) -> p e j", j=16)
        tt("dve", b3, base.unsqueeze(2).to_broadcast([128, 32, 16]), cr("thr", 3), ALU.is_gt, r=["base", "cR"], w=["big"])
        op("dve", lambda e: e.tensor_reduce(out=nblk, in_=b3, axis=AX.X, op=ALU.add), r=["big"], w=["nblk"])
        ts("dve", nblk, nblk, float(SLOTR), None, ALU.mult, None, r=["nblk"], w=["nblk"])
        b4 = big[:, 0:1024].rearrange("p (e f) -> p e f", f=32)
        tt("dve", b4, nblk.unsqueeze(1).to_broadcast([128, 32, 32]), cr("le", 3), ALU.mult, r=["nblk", "cR"], w=["big"])
        op("dve", lambda e: e.tensor_reduce(out=pend, in_=b4, axis=AX.X, op=ALU.add), r=["big"], w=["pend"])
        tt("dve", pstart, pend, nblk, ALU.subtract, r=["pend", "nblk"], w=["pstart"])
        b5 = big.rearrange("p (s e) -> p s e", e=32)
        tt("dve", b5, pend.unsqueeze(1).to_broadcast([128, NSLOT, 32]), cr("slotv", 3), ALU.is_le, r=["pend", "cR"], w=["big"])
        op("dve", lambda e: e.tensor_reduce(out=esl, in_=b5, axis=AX.X, op=ALU.add), r=["big"], w=["esl"])
        ts("dve", esl, esl, 31.0, None, ALU.min, None, r=["esl"], w=["esl"])
        pidx = ca("pidx")
        ts("dve", idxf[:, :, 0], esl, 128.0, pidx, ALU.mult, ALU.add, r=["esl", "cA"], w=["idxf"])
        for hc in range(4):
            ts("dve", idxf[:, :, 1 + hc], esl, 512.0, pidx, ALU.mult, ALU.add, r=["esl", "cA"], w=["idxf"])
            ts("dve", idxf[:, :, 1 + hc], idxf[:, :, 1 + hc], float(hc * 128), None, ALU.add, None, r=["idxf"], w=["idxf"])
        cp("dve", idxw.rearrange("p (s k) -> p s k", k=5), idxf, r=["idxf"], w=["idxw"])
        dtmp = alloc(NT * 2, F32).rearrange("p (t k) -> p t k", t=NT)
        bigv = big.rearrange("p (s e) -> p s e", e=32)
        tt("dve", bigv, oh_all.rearrange("p t k e -> p (t k) e"), pstart.unsqueeze(1).to_broadcast([128, NT * 2, 32]), ALU.mult,
           r=["pstart"], w=["big"])
        op("dve", lambda e: e.tensor_reduce(out=dtmp.rearrange("p t k -> p (t k)"), in_=bigv, axis=AX.X, op=ALU.add), r=["big"], w=["dtmp"])
        tt("dve", dtmp, dtmp, rk_all, ALU.add, r=["dtmp", "rk"], w=["dtmp"])
        cp("dve", destv, dtmp, r=["dtmp"], w=["dest"])
        for tile_i in range(NT):
            for k_ in range(2):
                S.add("pool", lambda e, tile_i=tile_i, k_=k_: e.indirect_dma_start(
                    out=xb_d, out_offset=bass.IndirectOffsetOnAxis(ap=destv[:, tile_i, k_:k_ + 1], axis=0),
                    in_=h2all[:, tile_i, :], in_offset=None), r=["dest", ("h2all", tile_i)], w=["xb"], dma=True)

        assert bgpos[0] == 96, bgpos
        S.barrier(include_bg=True)
        st["off"] = st["pers"]
        xblk = [alloc(D, BF16) for _ in range(4)]
        xT = [alloc(8 * 256, BF16).rearrange("p (k r) -> p k r", k=8) for _ in range(2)]
        W1 = [alloc(8 * 512, BF16).rearrange("p (k n) -> p k n", k=8) for _ in range(3)]
        W3 = [alloc(8 * 512, BF16).rearrange("p (k n) -> p k n", k=8) for _ in range(3)]
        W2 = [alloc(4 * D, BF16).rearrange("p (k n) -> p k n", k=4) for _ in range(3)]
        sg1 = [alloc(512, F32), alloc(512, F32)]
        a1 = [alloc(512, F32), alloc(512, F32)]
        actT = [alloc(4 * 256, BF16).rearrange("p (k r) -> p k r", k=4) for _ in range(2)]
        yst = [alloc(D, F32) for _ in range(4)]
        idxv = idxw.rearrange("p (s k) -> p s k", k=5)

        def stageA(s):
            wb = s % 3
            xb_ = s % 2
            S.add("pool", lambda e: e.indirect_dma_start(out=W1[wb].rearrange("p k n -> p (k n)"), out_offset=None, in_=ew1b_d,
                  in_offset=bass.IndirectOffsetOnAxis(ap=idxv[:, s, 0:1], axis=0)), r=["idxw"], w=[("W1", wb)], dma=True)
            S.add("pool", lambda e: e.indirect_dma_start(out=W3[wb].rearrange("p k n -> p (k n)"), out_offset=None, in_=ew3b_d,
                  in_offset=bass.IndirectOffsetOnAxis(ap=idxv[:, s, 0:1], axis=0)), r=["idxw"], w=[("W3", wb)], dma=True)
            for hc in range(4):
                S.add("pool", lambda e, hc=hc: e.indirect_dma_start(out=W2[wb][:, hc, :], out_offset=None, in_=ew2b_d,
                      in_offset=bass.IndirectOffsetOnAxis(ap=idxv[:, s, 1 + hc:2 + hc], axis=0)), r=["idxw"], w=[("W2", wb)], dma=True)
            for rt in range(2):
                r0 = s * SLOTR + rt * 128
                xi = xb_ * 2 + rt
                dma("sp", xblk[xi], xb_d[r0:r0 + 128, :], r=["xb"], w=[("xblk", xi)])
                bk = nb()
                xv = xblk[xi].rearrange("p (f k) -> p k f", k=8)
                op("pe", trgroup([(psb[bk][:, kk * 128:(kk + 1) * 128], xv[:, kk, :]) for kk in range(8)], identb),
                   r=[("xblk", xi), "cB"], w=[("ps", bk)])
                cp("act", xT[xb_][:, :, rt * 128:(rt + 1) * 128], psb[bk][:, :].rearrange("p (k r) -> p k r", k=8), r=[("ps", bk)], w=[("xT", xb_)])

        def stageB(s):
            wb = s % 3
            xb_ = s % 2
            for hp in range(2):
                b1_, b3_ = nb(), nb()
                l1_, l3_ = [], []
                for hh in range(2):
                    hc = hp * 2 + hh
                    for kk in range(8):
                        l1_.append((ps[b1_][:, hh * 256:(hh + 1) * 256], W1[wb][:, kk, hc * 128:(hc + 1) * 128], xT[xb_][:, kk, :], kk == 0, kk == 7))
                        l3_.append((ps[b3_][:, hh * 256:(hh + 1) * 256], W3[wb][:, kk, hc * 128:(hc + 1) * 128], xT[xb_][:, kk, :], kk == 0, kk == 7))
                op("pe", mmgroup(l1_), r=[("W1", wb), ("xT", xb_)], w=[("ps", b1_)])
                op("pe", mmgroup(l3_), r=[("W3", wb), ("xT", xb_)], w=[("ps", b3_)])
                act(sg1[hp], ps[b1_][:, :], AF.Sigmoid, r=[("ps", b1_)], w=[("sg1", hp)])
                tt("dve", a1[hp], ps[b1_][:, :], sg1[hp], ALU.mult, r=[("ps", b1_), ("sg1", hp)], w=[("a1", hp)])
                tt("dve", actT[xb_][:, hp * 2:hp * 2 + 2, :], a1[hp].rearrange("p (k r) -> p k r", k=2), ps[b3_][:, :].rearrange("p (k r) -> p k r", k=2),
                   ALU.mult, r=[("a1", hp), ("ps", b3_)], w=[("actT", xb_)])

        def stageC(s):
            wb = s % 3
            xb_ = s % 2
            for rt in range(2):
                yi = xb_ * 2 + rt
                for nh in range(2):
                    bk = nb()
                    op("pe", mmgroup([(ps[bk][:, :], actT[xb_][:, hc, rt * 128:(rt + 1) * 128], W2[wb][:, hc, nh * 512:(nh + 1) * 512], hc == 0, hc == 3)
                                      for hc in range(4)]), r=[("actT", xb_), ("W2", wb)], w=[("ps", bk)])
                    cp("act" if nh == 0 else "dve", yst[yi][:, nh * 512:(nh + 1) * 512], ps[bk][:, :], r=[("ps", bk)], w=[("yst", yi)])
                r0 = s * SLOTR + rt * 128
                dma("sp", yb_d[r0:r0 + 128, :], yst[yi], r=[("yst", yi)], w=["yb"])
        stageA(0)
        for s in range(NSLOT):
            if s + 1 < NSLOT:
                stageA(s + 1)
            stageB(s)
            if s >= 1:
                stageC(s - 1)
        stageC(NSLOT - 1)

        phase_reset()
        r1 = [alloc(D, F32) for _ in range(4)]
        r2 = [alloc(D, F32) for _ in range(4)]
        x2r = [alloc(D, F32) for _ in range(4)]
        yo = [alloc(D, F32) for _ in range(4)]
        ss3 = alloc(4, F32)
        junk3 = alloc(D, BF16)
        GT2 = alloc(D, F32)
        GF = alloc(D, F32)
        dma("sp", GT2, mod_d[:, 5120:6144], w=["modL"])
        dma("sp", GF, gains_d[:, 2048:3072], w=["gains"])
        outs = []
        for tile_i in range(NT):
            b = tile_i % 4
            r0 = tile_i * 128
            S.add("pool", lambda e, tile_i=tile_i, b=b: e.indirect_dma_start(out=r1[b], out_offset=None, in_=yb_d,
                  in_offset=bass.IndirectOffsetOnAxis(ap=destv[:, tile_i, 0:1], axis=0)), r=["yb", "dest"], w=[("r1", b)], dma=True)
            S.add("pool", lambda e, tile_i=tile_i, b=b: e.indirect_dma_start(out=r2[b], out_offset=None, in_=yb_d,
                  in_offset=bass.IndirectOffsetOnAxis(ap=destv[:, tile_i, 1:2], axis=0)), r=["yb", "dest"], w=[("r2", b)], dma=True)
            dma("sp", x2r[b], x2_d[r0:r0 + 128, :], w=[("x2r", b)])
            ts("dve", yo[b], r1[b], wtsv[:, tile_i, 0:1], None, ALU.mult, None, r=[("r1", b), "wts"], w=[("yo", b)])
            stt("dve", yo[b], r2[b], wtsv[:, tile_i, 1:2], yo[b], ALU.mult, ALU.add, r=[("r2", b), ("yo", b), "wts"], w=[("yo", b)])
            tt("dve", yo[b], yo[b], GT2, ALU.mult, r=[("yo", b), "modL"], w=[("yo", b)])
            tt("dve", yo[b], yo[b], x2r[b], ALU.add, r=[("yo", b), ("x2r", b)], w=[("yo", b)])
            sscol = ss3[:, b:b + 1]
            act(junk3, yo[b], AF.Square, r=[("yo", b)], w=["junk3", ("ss3", b)], accum=sscol)
            rstd_from_ss(sscol, D, ("ss3", b))
            stt("dve", yo[b], yo[b], sscol, GF, ALU.mult, ALU.mult, r=[("yo", b), ("ss3", b), "gains"], w=[("yo", b)])
            outs.append(dma("sp", out_d[r0:r0 + 128, :], yo[b], r=[("yo", b)]))
        S.emit(final_wait_ops=outs)
    return nc


def _bc(v, n=128):
    return np.ascontiguousarray(np.broadcast_to(np.asarray(v, np.float32).reshape(1, -1), (n, np.asarray(v).size)))


def _fm(v):
    v = np.asarray(v, np.float32).reshape(-1)
    return np.ascontiguousarray(v.reshape(-1, 128).T)


def make_in_maps(inp, cores):
    hc = host_consts()
    g = {k: np.asarray(v) for k, v in inp.items()}
    shared = dict(
        ada_w=np.ascontiguousarray(g["ada_w"][0]),
        ada_b_b=_bc(g["ada_b"][0]),
        gains_b=np.concatenate([_bc(g["norm1_g"][0]), _bc(g["norm2_g"][0]), _bc(g["final_norm_g"])], 1),
        w_in=np.ascontiguousarray(g["w_in"][0]),
        b_in_fm=_fm(g["b_in"][0]),
        b_in_b=_bc(g["b_in"][0][512:3072]),
        dlog_b=_bc(g["ret_decay_logit"][0].reshape(-1)),
        ret_w_o=np.ascontiguousarray(g["ret_w_o"][0]),
        hy_w_o=np.ascontiguousarray(g["hy_w_o"][0]),
        w_out=np.ascontiguousarray(g["w_out"][0]),
        hyskip=_fm(g["hy_skip"][0]),
        hw1=np.ascontiguousarray(g["hy_ffn_w1"][0]),
        hvec=np.ascontiguousarray(np.stack([g["hy_ffn_b1"][0], g["hy_ffn_freq"][0], g["hy_ffn_b2"][0], np.zeros(64, np.float32)], 1)),
        hw2=np.ascontiguousarray(g["hy_ffn_w2"][0]),
        hw3=np.ascontiguousarray(g["hy_ffn_w3"][0]),
        wr=np.ascontiguousarray(np.concatenate([g["router_group_w"][0], g["router_expert_w"][0]], 1)),
        br_b=_bc(np.concatenate([g["router_group_b"][0], g["router_expert_b"][0]])),
        ew1=np.ascontiguousarray(g["expert_w1"][0].reshape(32 * 128, 8 * 512)),
        ew3=np.ascontiguousarray(g["expert_w3"][0].reshape(32 * 128, 8 * 512)),
        ew2=np.ascontiguousarray(g["expert_w2"][0].reshape(32 * 512, D)),
        cA=hc["cA"], cR=hc["cR"], cB=hc["cB"], ftab=hc["ftab"], itab=hc["itab"], rope=hc["rope"],
        featsT=hc["featsT"], tvec=hc["tvec"],
    )
    cw = g["hy_conv_w"][0]
    cbias = g["hy_conv_b"][0]
    hyc = np.zeros((128, 12, 5), np.float32)
    for j in range(3):
        hyc[:, :, j] = cw[j].reshape(12, 128).T
    hyc[:, :, 3] = cbias.reshape(12, 128).T
    shared["hyc"] = hyc.reshape(128, 60)
    maps = []
    for b in cores:
        m = dict(shared)
        m["x"] = np.ascontiguousarray(g["x"][b])
        m["ctx"] = np.ascontiguousarray(g["ctx"][b])
        cv = np.zeros((128, 8, 2), np.float32)
        cv[:, :, 0] = g["c"][b].reshape(8, 128).T
        cv[:, :, 1] = g["c_ctx"].reshape(8, 128).T
        m["cvec"] = cv.reshape(128, 16)
        maps.append(m)
    return maps


def kernel(**inputs):
    nc = build_program(debug=False)
    maps = make_in_maps(inputs, list(range(8)))
    res = run_bass_kernel_spmd(nc, maps, core_ids=list(range(8)))
    return np.stack([np.asarray(r["out"], np.float32) for r in res.results], 0)
```
